# Optimizing a Trainium2 kernel written in Bass

```python
import jax
import jax.numpy as jnp
from jax import lax
import numpy as np

D_MODEL = 1024
BATCH = 32
SEQ = 2048
DEPTH = 4

MLSTM_HEADS = 4
MLSTM_DH = 64
MLSTM_CHUNK = 64
CONV_K = 4
HGRN_HEADS = 4
HGRN_DK = 64
HGRN_DV = 64
HGRN_CHUNK = 32
SWA_Q_HEADS = 8
SWA_KV_HEADS = 2
SWA_DH = 64
WINDOW = 128
ROPE_THETA = 500000.0
ROT_DIM = SWA_DH // 4
D_FF = 4 * D_MODEL
PLE_DIM = 256
EPS = 1e-6

MLSTM_W = MLSTM_HEADS * MLSTM_DH
HGRN_KW = HGRN_HEADS * HGRN_DK
HGRN_VW = HGRN_HEADS * HGRN_DV
SWA_QW = SWA_Q_HEADS * SWA_DH
SWA_KVW = SWA_KV_HEADS * SWA_DH
MIX_W = MLSTM_W + HGRN_VW + SWA_QW
IN_SPLITS = (MLSTM_W, MLSTM_W, MLSTM_W, MLSTM_W, MLSTM_HEADS, MLSTM_HEADS,
             HGRN_KW, HGRN_KW, HGRN_VW, HGRN_VW, SWA_QW, SWA_KVW, SWA_KVW)
IN_W = 4 * MLSTM_W + 2 * MLSTM_HEADS + 2 * HGRN_KW + 2 * HGRN_VW + SWA_QW + 2 * SWA_KVW

kernel_name = "hybrid_mlstm_hgrn2_swa_trunk"


def _rms_norm(x, g):
    xf = x.astype(jnp.float32)
    y = xf * lax.rsqrt(jnp.mean(xf * xf, axis=-1, keepdims=True) + EPS) * g.astype(jnp.float32)
    return y.astype(x.dtype)


def _head_rms_norm(x, g, n_heads):
    B, S, W = x.shape
    d = W // n_heads
    y = _rms_norm(x.reshape(B, S, n_heads, d), g.reshape(n_heads, d))
    return y.reshape(B, S, W)


def _split_cols(y, sizes):
    out = []
    o = 0
    for s in sizes:
        out.append(y[..., o:o + s])
        o += s
    return out


def _heads(t, n_heads):
    B, S, W = t.shape
    return t.reshape(B, S, n_heads, W // n_heads)


def _to_chunks(t, L):
    B, S, H = t.shape[:3]
    t = t.reshape((B, S // L, L, H) + t.shape[3:])
    return jnp.moveaxis(t, (1, 3), (0, 2))


def _from_chunks(t):
    nc, B, H, L, d = t.shape
    return jnp.moveaxis(t, (0, 2), (1, 3)).reshape(B, nc * L, H * d)


def _causal_conv(u, w, b):
    K = w.shape[0]
    y = lax.conv_general_dilated(u, w[:, None, :].astype(u.dtype), window_strides=(1,),
                                 padding=[(K - 1, 0)], dimension_numbers=('NWC', 'WIO', 'NWC'),
                                 feature_group_count=u.shape[-1])
    return y + b.astype(u.dtype)


def _rope_tables(positions):
    inv_freq = ROPE_THETA ** (-jnp.arange(0, ROT_DIM, 2, dtype=jnp.float32) / ROT_DIM)
    ang = positions.astype(jnp.float32)[..., None] * inv_freq
    return jnp.cos(ang)[:, :, None, :], jnp.sin(ang)[:, :, None, :]


def _partial_rope(x, cos, sin):
    xf = x.astype(jnp.float32)
    half = ROT_DIM // 2
    x1, x2, rest = xf[..., :half], xf[..., half:ROT_DIM], xf[..., ROT_DIM:]
    y = jnp.concatenate([x1 * cos - x2 * sin, x2 * cos + x1 * sin, rest], axis=-1)
    return y.astype(x.dtype)


def mlstm_chunkwise(q, k, v, i_pre, f_pre):
    f32 = jnp.float32
    B, S, H, dk = q.shape
    dv = v.shape[-1]
    L = MLSTM_CHUNK
    xs = (_to_chunks(q.astype(f32), L), _to_chunks(k.astype(f32), L), _to_chunks(v.astype(f32), L),
          _to_chunks(i_pre.astype(f32), L), _to_chunks(jax.nn.log_sigmoid(f_pre.astype(f32)), L))
    causal = jnp.tril(jnp.ones((L, L), dtype=bool))

    def step(carry, inp):
        C, n, m = carry
        q_, k_, v_, i_, lf = inp
        b = jnp.cumsum(lf, axis=-1)
        dmat = jnp.where(causal, b[..., :, None] - b[..., None, :] + i_[..., None, :], -jnp.inf)
        inter = b + m[..., None]
        m_j = jnp.maximum(inter, jnp.max(dmat, axis=-1))
        w_intra = jnp.exp(dmat - m_j[..., None])
        w_inter = jnp.exp(inter - m_j)
        s = jnp.einsum('bhld,bhsd->bhls', q_, k_) * w_intra
        num = jnp.einsum('bhls,bhse->bhle', s, v_) + w_inter[..., None] * jnp.einsum('bhld,bhde->bhle', q_, C)
        den = jnp.sum(s, axis=-1) + w_inter * jnp.einsum('bhld,bhd->bhl', q_, n)
        h = num / jnp.maximum(jnp.abs(den), jnp.exp(-m_j))[..., None]
        m_new = m_j[..., -1]
        w_s = jnp.exp(b[..., -1:] - b + i_ - m_new[..., None])
        decay = jnp.exp(b[..., -1] + m - m_new)
        C_new = decay[..., None, None] * C + jnp.einsum('bhs,bhsd,bhse->bhde', w_s, k_, v_)
        n_new = decay[..., None] * n + jnp.einsum('bhs,bhsd->bhd', w_s, k_)
        return (C_new, n_new, m_new), h

    init = (jnp.zeros((B, H, dk, dv), f32), jnp.zeros((B, H, dk), f32), jnp.zeros((B, H), f32))
    _, h = lax.scan(step, init, xs)
    return _from_chunks(h)


def hgrn2_chunkwise(q, f_pre, inp, lb):
    f32 = jnp.float32
    B, S, H, dk = q.shape
    dv = inp.shape[-1]
    L = HGRN_CHUNK
    fp = f_pre.astype(f32)
    lb = lb.astype(f32)
    log_f = jnp.logaddexp(jnp.log(lb), jnp.log1p(-lb) + jax.nn.log_sigmoid(fp))
    key = (1.0 - lb) * jax.nn.sigmoid(-fp)
    qf = jax.nn.silu(q.astype(f32))
    xs = (_to_chunks(qf, L), _to_chunks(key, L), _to_chunks(inp.astype(f32), L), _to_chunks(log_f, L))
    causal = jnp.tril(jnp.ones((L, L), dtype=bool))[:, :, None]

    def step(S_state, chunk):
        q_, k_, v_, lf = chunk
        G = jnp.cumsum(lf, axis=2)
        inter = jnp.einsum('bhld,bhde->bhle', q_ * jnp.exp(G), S_state)
        diff = G[:, :, :, None, :] - G[:, :, None, :, :]
        decay = jnp.exp(jnp.where(causal, diff, -jnp.inf))
        A = jnp.einsum('bhld,bhlsd,bhsd->bhls', q_, decay, k_)
        o = inter + jnp.einsum('bhls,bhse->bhle', A, v_)
        G_last = G[:, :, -1]
        S_new = jnp.exp(G_last)[..., None] * S_state + jnp.einsum(
            'bhsd,bhse->bhde', k_ * jnp.exp(G_last[:, :, None] - G), v_)
        return S_new, o

    _, o = lax.scan(step, jnp.zeros((B, H, dk, dv), f32), xs)
    return _from_chunks(o)


def swa_sink_attention(q, k, v, sinks):
    B, S, HQ, d = q.shape
    HKV = k.shape[2]
    G = HQ // HKV
    W = WINDOW
    nb = S // W
    qb = q.reshape(B, nb, W, HKV, G, d)
    kb = k.reshape(B, nb, W, HKV, d)
    vb = v.reshape(B, nb, W, HKV, d)
    kk = jnp.concatenate([jnp.concatenate([jnp.zeros_like(kb[:, :1]), kb[:, :-1]], axis=1), kb], axis=2)
    vv = jnp.concatenate([jnp.concatenate([jnp.zeros_like(vb[:, :1]), vb[:, :-1]], axis=1), vb], axis=2)
    s = jnp.einsum('bnqhgd,bnkhd->bnhgqk', qb, kk).astype(jnp.float32) * (d ** -0.5)
    qi = jnp.arange(W)[:, None]
    ki = jnp.arange(2 * W)[None, :]
    blk = jnp.arange(nb)[:, None, None]
    mask = (ki > qi) & (ki <= qi + W) & ((blk > 0) | (ki >= W))
    s = jnp.where(mask[None, :, None, None], s, -jnp.inf)
    sink = sinks.astype(jnp.float32).reshape(HKV, G)[None, None, :, :, None, None]
    m = jnp.maximum(jnp.max(s, axis=-1, keepdims=True), sink)
    pexp = jnp.exp(s - m)
    probs = pexp / (jnp.sum(pexp, axis=-1, keepdims=True) + jnp.exp(sink - m))
    o = jnp.einsum('bnhgqk,bnkhd->bnqhgd', probs.astype(v.dtype), vv)
    return o.reshape(B, S, HQ * d)


def setup_inputs(seed: int = 0) -> dict:
    key = jax.random.key(seed)
    ks = jax.random.split(key, 24)
    f32 = jnp.float32
    nrm = lambda k, shape, scale: jax.random.normal(k, shape, f32) * scale
    x = nrm(ks[0], (BATCH, SEQ, D_MODEL), 1.0)
    p = nrm(ks[1], (DEPTH, BATCH, SEQ, PLE_DIM), 1.0)
    start = jax.random.randint(ks[2], (BATCH, 1), 0, 4096, dtype=jnp.int32)
    positions = (start + jnp.arange(SEQ, dtype=jnp.int32)[None, :]).astype(jnp.int32)
    return {
        "x": x,
        "p": p,
        "positions": positions,
        "in_norm_g": 1.0 + nrm(ks[3], (DEPTH, D_MODEL), 0.05),
        "w_in": nrm(ks[4], (DEPTH, D_MODEL, IN_W), D_MODEL ** -0.5),
        "b_in": nrm(ks[5], (DEPTH, IN_W), 0.02),
        "mlstm_f_bias": jnp.linspace(3.0, 6.0, MLSTM_HEADS, dtype=f32)[None, :] + nrm(ks[6], (DEPTH, MLSTM_HEADS), 0.1),
        "mlstm_conv_w": nrm(ks[7], (DEPTH, CONV_K, 2 * MLSTM_W), CONV_K ** -0.5),
        "mlstm_conv_b": nrm(ks[8], (DEPTH, 2 * MLSTM_W), 0.02),
        "mlstm_norm_g": 1.0 + nrm(ks[9], (DEPTH, MLSTM_W), 0.05),
        "hgrn_lb_logits": nrm(ks[10], (DEPTH, HGRN_KW), 0.1),
        "hgrn_norm_g": 1.0 + nrm(ks[11], (DEPTH, HGRN_VW), 0.05),
        "swa_q_norm_g": 1.0 + nrm(ks[12], (DEPTH, SWA_DH), 0.05),
        "swa_k_norm_g": 1.0 + nrm(ks[13], (DEPTH, SWA_DH), 0.05),
        "swa_sinks": nrm(ks[14], (DEPTH, SWA_Q_HEADS), 0.5),
        "w_out": nrm(ks[15], (DEPTH, MIX_W, D_MODEL), MIX_W ** -0.5),
        "mlp_norm_g": 1.0 + nrm(ks[16], (DEPTH, D_MODEL), 0.05),
        "w_up": nrm(ks[17], (DEPTH, D_MODEL, D_FF), D_MODEL ** -0.5),
        "w_down": nrm(ks[18], (DEPTH, D_FF, D_MODEL), D_FF ** -0.5),
        "ple_norm_g": 1.0 + nrm(ks[19], (DEPTH, D_MODEL), 0.05),
        "w_ple_gate": nrm(ks[20], (DEPTH, D_MODEL, D_MODEL), D_MODEL ** -0.5),
        "w_ple_proj": nrm(ks[21], (DEPTH, PLE_DIM, D_MODEL), PLE_DIM ** -0.5),
        "ple_post_norm_g": 1.0 + nrm(ks[22], (DEPTH, D_MODEL), 0.05),
    }


def reference(x, p, positions, in_norm_g, w_in, b_in, mlstm_f_bias, mlstm_conv_w, mlstm_conv_b,
              mlstm_norm_g, hgrn_lb_logits, hgrn_norm_g, swa_q_norm_g, swa_k_norm_g, swa_sinks,
              w_out, mlp_norm_g, w_up, w_down, ple_norm_g, w_ple_gate, w_ple_proj, ple_post_norm_g):
    cos, sin = _rope_tables(positions)
    lb_all = jnp.cumsum(jax.nn.softmax(hgrn_lb_logits.astype(jnp.float32), axis=0), axis=0)
    lb_all = lb_all - lb_all[0:1]
    B, S, _ = x.shape
    for l in range(DEPTH):
        h = _rms_norm(x, in_norm_g[l])
        y = h @ w_in[l] + b_in[l]
        (mq, mk, mv, mo, mi, mf, hq, hf, hi, hg, sq, sk, sv) = _split_cols(y, IN_SPLITS)

        qk = jax.nn.silu(_causal_conv(jnp.concatenate([mq, mk], axis=-1), mlstm_conv_w[l], mlstm_conv_b[l]))
        mq_c, mk_c = qk[..., :MLSTM_W], qk[..., MLSTM_W:]
        m_h = mlstm_chunkwise(_heads(mq_c, MLSTM_HEADS), _heads(mk_c, MLSTM_HEADS) * (MLSTM_DH ** -0.5),
                              _heads(mv, MLSTM_HEADS), mi, mf + mlstm_f_bias[l])
        m_out = (_head_rms_norm(m_h, mlstm_norm_g[l], MLSTM_HEADS)
                 * jax.nn.sigmoid(mo.astype(jnp.float32))).astype(x.dtype)

        h_h = hgrn2_chunkwise(_heads(hq, HGRN_HEADS), _heads(hf, HGRN_HEADS), _heads(hi, HGRN_HEADS),
                              lb_all[l].reshape(HGRN_HEADS, HGRN_DK))
        h_out = (_head_rms_norm(h_h, hgrn_norm_g[l], HGRN_HEADS)
                 * jax.nn.silu(hg.astype(jnp.float32))).astype(x.dtype)

        q_s = _partial_rope(_rms_norm(_heads(sq, SWA_Q_HEADS), swa_q_norm_g[l]), cos, sin)
        k_s = _partial_rope(_rms_norm(_heads(sk, SWA_KV_HEADS), swa_k_norm_g[l]), cos, sin)
        s_out = swa_sink_attention(q_s, k_s, _heads(sv, SWA_KV_HEADS), swa_sinks[l]).astype(x.dtype)

        x = x + jnp.concatenate([m_out, h_out, s_out], axis=-1) @ w_out[l]

        u = _rms_norm(x, mlp_norm_g[l]) @ w_up[l]
        x = x + jnp.square(jax.nn.relu(u)) @ w_down[l]

        gate = jax.nn.sigmoid((_rms_norm(x, ple_norm_g[l]) @ w_ple_gate[l]).astype(jnp.float32))
        e = _rms_norm(p[l] @ w_ple_proj[l], ple_post_norm_g[l]).astype(jnp.float32)
        x = x + (gate * e).astype(x.dtype)
    return x
```

```python
import contextlib
import math

import numpy as np
import concourse.bass as bass
import concourse.mybir as mybir
from concourse.bass_utils import run_bass_kernel_spmd

F32 = mybir.dt.float32
BF16 = mybir.dt.bfloat16
I32 = mybir.dt.int32
AF = mybir.ActivationFunctionType
ALU = mybir.AluOpType
AX = mybir.AxisListType

N_CORES = 8
D = 1024
KC = 8
DFF = 4096
PLE = 256
IN_W = 2824
EPS = 1e-6
ROPE_THETA = 500000.0
NEG = -30000.0
TWO_PI = 2.0 * math.pi

O_MQ, O_MK, O_MV, O_MO, O_MI, O_MF = 0, 256, 512, 768, 1024, 1028
O_HQ, O_HF, O_HI, O_HG = 1032, 1288, 1544, 1800
O_SQ, O_SK, O_SV = 2056, 2568, 2696
QPERM = [0, 4, 1, 5, 2, 6, 3, 7]
A_PIECES = [(0, O_MV, 256), (256, O_MO, 256), (512, O_HI, 256), (768, O_HG, 256)]
A_PIECES += [(1024 + j * 64, O_SQ + QPERM[j] * 64, 64) for j in range(8)]
A_PIECES += [(1536, O_SK, 128), (1664, O_SV, 128), (1792, O_MI, 4), (1796, O_MF, 4)]
A_W = 1800
B_PIECES = [(0, O_MQ, 256), (256, O_MK, 256), (512, O_HQ, 256), (768, O_HF, 256)]
B_W = 1024

ENGS = ("pe", "act", "dve", "pool", "sp")


class Tracker:
    def __init__(self):
        self.streams = {e: [] for e in ENGS}
        self.count = {e: 0 for e in ENGS}
        self.waited = {e: {} for e in ENGS}
        self.last_w = {}
        self.readers = {}
        self.dma_cum = {}

    def _need(self, eng, deps, pe_skip=True):
        best = {}
        for d in deps:
            if d is None:
                continue
            kind, key, n = d
            if kind == "e" and key == eng and eng == "pe" and pe_skip:
                continue
            if kind == "d":
                n = self.dma_cum[key]
            k = (kind, key)
            if n > best.get(k, 0):
                best[k] = n
        for k, n in best.items():
            if n > self.waited[eng].get(k, 0):
                self.waited[eng][k] = n
                self.streams[eng].append(("wait", k, n))

    def _deps_for(self, reads, writes):
        deps = []
        for r in reads:
            deps.append(self.last_w.get(r))
        for w in writes:
            deps.append(self.last_w.get(w))
            deps.extend(self.readers.get(w, ()))
        return deps

    def _commit(self, me, reads, writes):
        for r in reads:
            self.readers.setdefault(r, []).append(me)
        for w in writes:
            self.last_w[w] = me
            self.readers[w] = []

    def op(self, eng, fn, reads=(), writes=()):
        self._need(eng, self._deps_for(reads, writes))
        self.count[eng] += 1
        me = ("e", eng, self.count[eng])
        self.streams[eng].append(("ins", fn, None))
        self._commit(me, reads, writes)

    def dma(self, q, fn, slot, reads=(), writes=()):
        self._need(q, self._deps_for(reads, writes))
        self.dma_cum[slot] = self.dma_cum.get(slot, 0) + 16
        me = ("d", slot, self.dma_cum[slot])
        self.streams[q].append(("dma", fn, slot))
        self._commit(me, reads, writes)

    def final_wait(self, eng, resources):
        deps = []
        for r in resources:
            deps.append(self.last_w.get(r))
            deps.extend(self.readers.get(r, ()))
        self._need(eng, deps, pe_skip=False)

    def emit(self, nc, stack, epoch=16000):
        sems = {}
        for e in ENGS:
            for k in range(self.count[e] // epoch + 1):
                sems[("e", e, k)] = stack.enter_context(nc.semaphore("s_%s%d" % (e, k)))
        for i, slot in enumerate(self.dma_cum):
            sems[("d", slot)] = stack.enter_context(nc.semaphore("d%d" % i))
        block = stack.enter_context(nc.Block())
        hmap = {"pe": block.tensor, "act": block.scalar, "dve": block.vector,
                "pool": block.gpsimd, "sp": block.sync}

        def make(e):
            def body(h):
                n_ins = 0
                for kind, a, b in self.streams[e]:
                    if kind == "wait":
                        if a[0] == "e":
                            h.wait_ge(sems[("e", a[1], (b - 1) // epoch)], (b - 1) % epoch + 1)
                        else:
                            h.wait_ge(sems[a], b)
                    elif kind == "ins":
                        a(h).then_inc(sems[("e", e, n_ins // epoch)], 1)
                        n_ins += 1
                    else:
                        a(h).then_inc(sems[("d", b)], 16)
            return body

        for e in ENGS:
            if self.streams[e]:
                hmap[e](make(e))


def build_program(cfg):
    S = cfg["S"]
    SEG = cfg["SEG"]
    GT = cfg["GT"]
    DEPTH = cfg["DEPTH"]
    NSEQ = cfg["NSEQ"]
    T_ = SEG // 128
    NSEGS = S // SEG
    NG = T_ // GT
    GN = GT * 128
    MG = min(4, T_)
    MGN = MG * 128
    NTOK = NSEQ * S

    nc = bass.Bass("TRN2", target_bir_lowering=False)
    dram = {}

    def din(name, shape, dt=F32):
        dram[name] = nc.dram_tensor(name, list(shape), dt, kind="ExternalInput").ap()
        return dram[name]

    x_d = din("x", [NTOK, D])
    p_d = din("p", [DEPTH, NTOK, PLE])
    pos_d = din("positions", [NTOK], I32)
    in_norm_g = din("in_norm_g", [DEPTH, D])
    w_in = din("w_in", [DEPTH, D, IN_W])
    b_in = din("b_in", [DEPTH, IN_W])
    f_bias = din("mlstm_f_bias", [DEPTH, 4])
    conv_w = din("mlstm_conv_w", [DEPTH, 4, 512])
    conv_b = din("mlstm_conv_b", [DEPTH, 512])
    m_norm_g = din("mlstm_norm_g", [DEPTH, 256])
    lb_logits = din("hgrn_lb_logits", [DEPTH, 256])
    h_norm_g = din("hgrn_norm_g", [DEPTH, 256])
    q_norm_g = din("swa_q_norm_g", [DEPTH, 64])
    k_norm_g = din("swa_k_norm_g", [DEPTH, 64])
    sinks_d = din("swa_sinks", [DEPTH, 8])
    w_out = din("w_out", [DEPTH, D, D])
    mlp_norm_g = din("mlp_norm_g", [DEPTH, D])
    w_up = din("w_up", [DEPTH, D, DFF])
    w_down = din("w_down", [DEPTH, DFF, D])
    ple_norm_g = din("ple_norm_g", [DEPTH, D])
    w_gate = din("w_ple_gate", [DEPTH, D, D])
    w_proj = din("w_ple_proj", [DEPTH, PLE, D])
    post_g = din("ple_post_norm_g", [DEPTH, D])
    out_d = nc.dram_tensor("out", [NTOK, D], F32, kind="ExternalOutput").ap()

    T = Tracker()
    st = contextlib.ExitStack()

    NBLK = 14
    wblk = nc.dram_tensor("wblk", [DEPTH, NBLK, 128, KC * 1024], BF16, kind="Internal").ap()

    def sb(name, shape, dt=F32):
        return st.enter_context(nc.sbuf_tensor(name, list(shape), dt))

    xres = sb("xres", [128, T_, D])
    slots = [sb("slot%d" % i, [128, KC, 1024], BF16) for i in range(4)]
    hT = sb("hT", [128, KC, SEG], BF16)
    xn = sb("xn", [128, D], BF16)
    PREW = max(GN, MGN) + 3
    pre = sb("pre", [128, 4, PREW])
    GNA = max(GN, 512)
    acc = sb("acc", [128, GNA])
    qkc = sb("qkc", [128, 4, GN], BF16)
    hq_s = sb("hq_s", [128, 2, GNA])
    Gc = sb("Gc", [128, 2, GN + 1])
    key = sb("key", [128, 2, GNA])
    ftmp = sb("ftmp", [128, GNA])
    tails = sb("tails", [128, DEPTH, 4, 3])
    og = sb("og", [128, 256])
    hgs = sb("hgs", [128, 256])
    Vm = sb("Vm", [128, 4, 65], BF16)
    Vt = sb("Vt", [128, 4, 65], BF16)
    Vh0 = sb("Vh0", [128, 256], BF16)
    Vh1 = sb("Vh1", [128, 256], BF16)
    sq2 = sb("sq2", [128, 2, 10, 64])
    sqk = sq2[:, 0]
    sqt = sq2[:, 1]
    gt = sb("gt", [128, 8])
    grep = sq2[:].rearrange("p a h d -> p (a h d)")[:, 0:KC * 128].rearrange("p (c t) -> p c t", t=128)
    Vs = sb("Vs", [128, T_ + 1, 2, 65], BF16)
    KT = sb("KT", [128, (T_ + 1) * 128], BF16)
    KTcar = sb("KTcar", [128, DEPTH, 128], BF16)
    Vscar = sb("Vscar", [128, DEPTH, 2, 65], BF16)
    QT0 = sb("QT0", [128, 512], BF16)
    QT1 = sb("QT1", [128, 512], BF16)
    Pt = [sb("Pt%d" % i, [128, 512], BF16) for i in range(2)]
    Wt = sb("Wt", [128, 512], BF16)
    At = sb("At", [128, 256], BF16)
    k_tm = sb("k_tm", [128, 2, 128], BF16)
    kh_tm = sb("kh_tm", [128, 2, 128], BF16)
    qbd_m = sb("qbd_m", [128, 2, 256], BF16)
    qh = sb("qh", [128, 2, 128], BF16)
    kh = sb("kh", [128, 2, 128], BF16)
    qbd_h = sb("qbd_h", [128, 2, 2, 128], BF16)
    E1 = sb("E1", [128, 64])
    E2 = sb("E2", [128, 64])
    mix = sb("mix", [128, D], BF16)
    mixT = xn[:].rearrange("p (c t) -> p c t", t=128)
    mC = sb("mC", [128, DEPTH, 2, 65])
    mCbd = sb("mCbd", [128, DEPTH, 2, 130], BF16)
    tmpC = sb("tmpC", [128, 2, 65])
    hS = sb("hS", [128, DEPTH, 2, 64])
    hSbd = sb("hSbd", [128, 2, 128], BF16)
    tmpS = sb("tmpS", [128, 2, 64])
    hr = sb("hr", [128, 4, 65])
    t256 = sb("t256", [128, 256])
    u256 = sb("u256", [128, 256])
    so = hr
    sm = sb("sm", [128, 64])
    sm2 = sb("sm2", [128, 64])
    hgE = sb("hgE", [128, 3, 2, 2])
    rope_c = sb("rope_c", [128, T_, 8])
    rope_s = sb("rope_s", [128, T_, 8])
    rtmp = sb("rtmp", [128, 4, 10, 8])
    qkr = sb("qkr", [128, 10, 64], BF16)
    ident_bf = sb("ident_bf", [128, 128], BF16)
    ident_f = sb("ident_f", [128, 128])
    tri_f = sb("tri_f", [128, 128])
    ones_f = sb("ones_f", [128, 128])
    onesb = sb("onesb", [128, 512], BF16)
    mask4 = sb("mask4", [128, 4, 128], BF16)
    mask64 = sb("mask64", [128, 4, 64], BF16)
    nm_cur = sb("nm_cur", [128, 4, 128], BF16)
    nm_prev = sb("nm_prev", [128, 4, 128], BF16)
    ones_row = onesb
    brow_a = sb("brow_a", [1, A_W], BF16)
    brow_b = sb("brow_b", [1, B_W], BF16)
    stg1 = t256
    stg2 = u256
    gcols = sb("gcols", [128, 3, DEPTH, KC])
    cwcol = sb("cwcol", [128, DEPTH, 4, 4])
    cbcol = sb("cbcol", [128, DEPTH, 4])
    lbcol = sb("lbcol", [128, DEPTH, 2])
    lbe = sb("lbe", [128, DEPTH, 2])
    omlb = sb("omlb", [128, DEPTH, 2])
    gm_b = sb("gm_b", [128, 256])
    gh_b = sb("gh_b", [128, 256])
    gqk_b = sb("gqk_b", [128, 10, 64])
    fb_b = sb("fb_b", [128, 4])
    esink = sb("esink", [128, 8])
    aT = pre[:].rearrange("p a b -> p (a b)").bitcast(BF16)[:, 0:KC * MGN].rearrange("p (c n) -> p c n", n=MGN)
    sqv = acc
    ptile = [t256, u256]
    gpost_b = key[:, :, 0:512].rearrange("p r n -> p (r n)")
    gate = hq_s[:, :, 0:512].rearrange("p r n -> p (r n)")
    etmp = ftmp[:, 0:512]
    zsrc_t = acc
    pbf = sb("pbf", [128, PLE], BF16)
    pT = sb("pT", [128, 2, 128], BF16)
    post = sb("post", [128, T_], I32)
    posf = sb("posf", [128, T_])
    angk = sb("angk", [128, T_, 8])
    angi = sb("angi", [128, T_, 8], I32)
    angf = sb("angf", [128, T_, 8])
    angm = sb("angm", [128, T_, 8])
    angw = sb("angw", [128, T_, 8])

    pf = [st.enter_context(nc.psum_tensor("pf%d" % i, [128, 512], F32)) for i in range(6)]
    pb = [st.enter_context(nc.psum_tensor("pb%d" % i, [128, 1024], BF16)) for i in range(2)]
    rr = {"f": 0, "b": 0}

    def bank_f():
        i = rr["f"] % 6
        rr["f"] += 1
        return pf[i], "pf%d" % i

    def bank_b():
        i = rr["b"] % 2
        rr["b"] += 1
        return pb[i], "pb%d" % i

    def mm(out, lhsT, rhs, start, stop, reads, writes):
        T.op("pe", lambda h: h.matmul(out, lhsT=lhsT, rhs=rhs, start=start, stop=stop), reads, writes)

    def tr(out, in_, ident, reads, writes):
        T.op("pe", lambda h: h.transpose(out, in_, ident), reads, writes)

    def act(out, in_, func, reads, writes, bias=None, scale=None, accum=None):
        kw = {}
        if bias is not None:
            kw["bias"] = bias
        if scale is not None:
            kw["scale"] = scale
        if accum is not None:
            kw["accum_out"] = accum
        T.op("act", lambda h: h.activation(out=out, in_=in_, func=func, **kw), reads, writes)

    def ts(eng, out, in0, s1, op0, reads, writes, s2=None, op1=None):
        if op1 is None:
            T.op(eng, lambda h: h.tensor_scalar(out=out, in0=in0, scalar1=s1, scalar2=None, op0=op0), reads, writes)
        else:
            T.op(eng, lambda h: h.tensor_scalar(out=out, in0=in0, scalar1=s1, scalar2=s2, op0=op0, op1=op1), reads, writes)

    def tt(eng, out, in0, in1, op, reads, writes):
        T.op(eng, lambda h: h.tensor_tensor(out=out, in0=in0, in1=in1, op=op), reads, writes)

    def stt(out, in0, scalar, in1, op0, op1, reads, writes):
        T.op("dve", lambda h: h.scalar_tensor_tensor(out=out, in0=in0, scalar=scalar, in1=in1, op0=op0, op1=op1), reads, writes)

    def cp(eng, out, in_, reads, writes):
        if eng == "act":
            T.op(eng, lambda h: h.activation(out=out, in_=in_, func=AF.Copy), reads, writes)
        else:
            T.op(eng, lambda h: h.tensor_copy(out=out, in_=in_), reads, writes)

    def memset(eng, ap, val, writes):
        T.op(eng, lambda h: h.memset(ap, val), (), writes)

    def recip(out, in_, reads, writes):
        T.op("dve", lambda h: h.reciprocal(out=out, in_=in_), reads, writes)

    def reduce_add(out, in_, reads, writes):
        T.op("dve", lambda h: h.tensor_reduce(out=out, in_=in_, axis=AX.X, op=ALU.add), reads, writes)

    def dma(q, out, in_, slot, reads, writes, slow=False):
        if slow:
            T.dma(q, lambda h: h.dma_start(out=out, in_=in_, allow_slow_non_contiguous=True), slot, reads, writes)
        else:
            T.dma(q, lambda h: h.dma_start(out=out, in_=in_), slot, reads, writes)

    def asel(out, in_, pattern, cmp_op, fill, base, cm, reads, writes):
        T.op("pool", lambda h: h.affine_select(out=out, in_=in_, pattern=pattern, compare_op=cmp_op,
                                               fill=fill, base=base, channel_multiplier=cm), reads, writes)

    memset("pool", onesb[:], 1.0, ["onesb"])
    memset("pool", acc[:], 0.0, ["acc"])
    memset("dve", ones_f[:], 1.0, ["ones_f"])
    ob4 = onesb[:].rearrange("p (a b) -> p a b", b=128)
    asel(tri_f[:], onesb[:, 0:128], [[1, 128]], ALU.is_ge, 0.0, 0, -1, ["onesb"], ["tri_f"])
    asel(mask4[:], ob4, [[0, 4], [1, 128]], ALU.is_ge, 0.0, 0, -1, ["onesb"], ["mask4"])
    asel(ident_f[:], onesb[:, 0:128], [[-1, 128]], ALU.is_equal, 0.0, 0, 1, ["onesb"], ["ident_f"])
    asel(ident_bf[:], onesb[:, 0:128], [[-1, 128]], ALU.is_equal, 0.0, 0, 1, ["onesb"], ["ident_bf"])
    ob64 = onesb[:, 0:256].rearrange("p (a b) -> p a b", b=64)
    for hp in range(2):
        asel(mask64[hp * 64:(hp + 1) * 64], ob64[hp * 64:(hp + 1) * 64], [[0, 4], [1, 64]], ALU.is_ge, 0.0, 0, -1,
             ["onesb"], ["mask64"])
    zsrc = zsrc_t[:, 0:512].rearrange("p (a b) -> p a b", b=128)
    asel(nm_cur[:], zsrc, [[0, 4], [1, 128]], ALU.is_ge, NEG, 0, -1, ["acc", "ftmp"], ["nm_cur"])
    asel(nm_prev[:], zsrc, [[0, 4], [-1, 128]], ALU.is_ge, NEG, -1, 1, ["acc", "ftmp"], ["nm_prev"])
    memset("dve", Vm[:], 1.0, ["Vm"])
    memset("dve", Vs[:], 1.0, ["Vs"])
    memset("dve", Vscar[:], 1.0, ["Vscar"])
    memset("dve", KTcar[:], 0.0, ["KTcar"])
    memset("pool", Vh0[:], 0.0, ["Vh0"])
    memset("pool", Vh1[:], 0.0, ["Vh1"])
    memset("pool", QT0[:], 0.0, ["QT0"])
    memset("pool", QT1[:], 0.0, ["QT1"])
    memset("pool", qbd_m[:], 0.0, ["qbd_m"])
    memset("pool", qbd_h[:], 0.0, ["qbd_h"])
    memset("pool", hSbd[:], 0.0, ["hSbd"])
    memset("pool", mCbd[:], 0.0, ["mCbd"])
    memset("dve", Gc[:], 0.0, ["Gc"])

    nrow1 = 3 * DEPTH * KC
    for i, src in enumerate((in_norm_g, mlp_norm_g, ple_norm_g)):
        dma("sp", stg1[i * DEPTH * KC:(i + 1) * DEPTH * KC, 0:128], src.rearrange("l (c p) -> (l c) p", p=128),
            "stg1", [], ["t256"])
    b1, b1n = bank_f()
    tr(b1[:, 0:nrow1], stg1[0:nrow1, 0:128], ident_f[0:nrow1, 0:nrow1], ["t256", "ident_f"], [b1n])
    cp("dve", gcols[:].rearrange("p a l c -> p (a l c)"), b1[:, 0:nrow1], [b1n], ["gcols"])
    n_cw = DEPTH * 4 * 4
    n_cb = DEPTH * 4
    n_lb = DEPTH * 2
    dma("sp", stg2[0:n_cw, 0:128], conv_w.rearrange("l j (c p) -> (l j c) p", p=128), "stg2", [], ["u256"])
    dma("sp", stg2[n_cw:n_cw + n_cb, 0:128], conv_b.rearrange("l (c p) -> (l c) p", p=128), "stg2", [], ["u256"])
    dma("sp", stg2[n_cw + n_cb:n_cw + n_cb + n_lb, 0:128], lb_logits.rearrange("l (c p) -> (l c) p", p=128),
        "stg2", [], ["u256"])
    nrow2 = n_cw + n_cb + n_lb
    b2, b2n = bank_f()
    tr(b2[:, 0:nrow2], stg2[0:nrow2, 0:128], ident_f[0:nrow2, 0:nrow2], ["u256", "ident_f"], [b2n])
    cp("dve", cwcol[:].rearrange("p l j c -> p (l j c)"), b2[:, 0:n_cw], [b2n], ["cwcol"])
    cp("dve", cbcol[:].rearrange("p l c -> p (l c)"), b2[:, n_cw:n_cw + n_cb], [b2n], ["cbcol"])
    cp("dve", lbcol[:].rearrange("p l c -> p (l c)"), b2[:, n_cw + n_cb:nrow2], [b2n], ["lbcol"])
    act(lbe[:], lbcol[:], AF.Exp, ["lbcol"], ["lbe"])
    reduce_add(sm[:, 0:2], lbe[:].rearrange("p l c -> p c l"), ["lbe"], ["sm"])
    recip(sm[:, 0:2], sm[:, 0:2], ["sm"], ["sm"])
    tt("dve", lbe[:], lbe[:], sm[:, 0:2].unsqueeze(1).to_broadcast([128, DEPTH, 2]), ALU.mult, ["lbe", "sm"], ["lbe"])
    memset("dve", lbcol[:, 0, :], 0.0, ["lbcol"])
    for l in range(1, DEPTH):
        tt("dve", lbcol[:, l, :], lbcol[:, l - 1, :], lbe[:, l, :], ALU.add, ["lbcol", "lbe"], ["lbcol"])
    ts("dve", omlb[:], lbcol[:], -1.0, ALU.mult, ["lbcol"], ["omlb"], 1.0, ALU.add)
    def layer_blocks(l):
        bl = []
        bl.append(("B", [(bo, n, w_in[l][:, oo:oo + n]) for (bo, oo, n) in B_PIECES], KC))
        bl.append(("A1", [(ao, n, w_in[l][:, oo:oo + n]) for (ao, oo, n) in A_PIECES if ao < 1024], KC))
        bl.append(("A2", [(ao - 1024, n, w_in[l][:, oo:oo + n]) for (ao, oo, n) in A_PIECES if ao >= 1024], KC))
        bl.append(("O", [(0, 1024, w_out[l][:, :])], KC))
        for j in range(4):
            bl.append(("U%d" % j, [(0, 1024, w_up[l][:, j * 1024:(j + 1) * 1024])], KC))
            bl.append(("D%d" % j, [(0, 1024, w_down[l][j * 1024:(j + 1) * 1024, :])], KC))
        bl.append(("G", [(0, 1024, w_gate[l][:, :])], KC))
        bl.append(("P", [(0, 1024, w_proj[l][:, :])], 2))
        return bl

    for l in range(DEPTH):
        for bi, (bname, pieces, nch) in enumerate(layer_blocks(l)):
            img = wblk[l, bi].rearrange("p (c n) -> p c n", n=1024)
            for (co, n, src) in pieces:
                T.dma("pool", lambda h, img=img, co=co, n=n, src=src, nch=nch: h.dma_start(
                    out=img[:, 0:nch, co:co + n], in_=src.rearrange("(c p) n -> p c n", p=128)),
                    "wblk%d" % l, [], ["wblk%d" % l])

    blocks = []
    for seq in range(NSEQ):
        for sg in range(NSEGS):
            for l in range(DEPTH):
                for bi, (bname, pieces, nch) in enumerate(layer_blocks(l)):
                    ncol = max(co + n for (co, n, _) in pieces)
                    blocks.append(((seq, sg, l, bname), l, bi, nch, ncol))
    ws = {"next": 0, "free": [True] * 4, "loc": {}}

    def pump():
        while ws["next"] < len(blocks):
            s = ws["next"] % 4
            if not ws["free"][s]:
                break
            key_, wl, bi, nch, ncol = blocks[ws["next"]]
            if ncol == 1024:
                dma("sp", slots[s][:, 0:nch, :].rearrange("p c n -> p (c n)"), wblk[wl, bi][:, 0:nch * 1024],
                    "slot%d" % s, ["wblk%d" % wl], ["slot%d" % s])
            else:
                dma("sp", slots[s][:, 0:nch, 0:ncol],
                    wblk[wl, bi].rearrange("p (c n) -> p c n", n=1024)[:, 0:nch, 0:ncol],
                    "slot%d" % s, ["wblk%d" % wl], ["slot%d" % s])
            ws["free"][s] = False
            ws["loc"][key_] = s
            ws["next"] += 1

    def wslot(key_):
        s = ws["loc"][key_]
        return slots[s], "slot%d" % s

    def release(key_):
        s = ws["loc"].pop(key_)
        ws["free"][s] = True
        pump()

    pump()

    inv_freq = [ROPE_THETA ** (-(2.0 * j) / 16.0) for j in range(8)]

    def norm_tiles(tiles, gsel, l, col0):
        cp("dve", grep, gcols[:, gsel, l, :].unsqueeze(2).to_broadcast([128, KC, 128]), ["gcols"], ["sqk", "sqt"])
        for i, t in enumerate(tiles):
            xr = "xres%d" % t
            act(xn[:], xres[:, t, :], AF.Square, [xr], ["xn", "sm"], accum=sm[:, 0:1])
            act(sm[:, 1:2], sm[:, 0:1], AF.Sqrt, ["sm"], ["sm"], bias=EPS, scale=1.0 / D)
            recip(sm[:, 2:3], sm[:, 1:2], ["sm"], ["sm"])
            act(xn[:], xres[:, t, :], AF.Copy, [xr, "sm"], ["xn"], scale=sm[:, 2:3])
            bk, bkn = bank_b()
            for c in range(KC):
                tr(bk[:, c * 128:(c + 1) * 128], xn[:, c * 128:(c + 1) * 128], ident_bf[:], ["xn", "ident_bf"], [bkn])
            c0 = col0 + i * 128
            tt("dve", hT[:, :, c0:c0 + 128], bk[:, :].rearrange("p (c t) -> p c t", t=128), grep, ALU.mult,
               [bkn, "sqk", "sqt"], ["hT"])

    for seq in range(NSEQ):
        for sg in range(NSEGS):
            tok0 = seq * S + sg * SEG
            first_seg = (sg == 0)
            for t in range(T_):
                dma("sp", xres[:, t, :], x_d[tok0 + t * 128: tok0 + (t + 1) * 128, :], "xres%d" % t,
                    [], ["xres%d" % t])
            dma("sp", post[:], pos_d[tok0:tok0 + SEG].rearrange("(t p) -> p t", p=128), "post", [], ["post"], slow=True)
            cp("dve", posf[:], post[:], ["post"], ["posf"])
            for j in range(8):
                ts("dve", angk[:, :, j], posf[:], float(np.float32(inv_freq[j])), ALU.mult, ["posf"], ["angk"])
            for shift, dst, dstn in ((0.0, rope_s, "rope_s"), (math.pi / 2.0, rope_c, "rope_c")):
                ts("dve", angm[:], angk[:], shift, ALU.add, ["angk"], ["angm"])
                ts("dve", angf[:], angm[:], 1.0 / TWO_PI, ALU.mult, ["angm"], ["angf"])
                cp("dve", angi[:], angf[:], ["angf"], ["angi"])
                cp("dve", angf[:], angi[:], ["angi"], ["angf"])
                stt(angf[:], angf[:], -TWO_PI, angm[:], ALU.mult, ALU.add, ["angf", "angm"], ["angf"])
                ts("dve", angw[:], angf[:], math.pi, ALU.is_gt, ["angf"], ["angw"], -TWO_PI, ALU.mult)
                tt("dve", angf[:], angf[:], angw[:], ALU.add, ["angf", "angw"], ["angf"])
                ts("dve", angw[:], angf[:], -math.pi, ALU.is_lt, ["angf"], ["angw"], TWO_PI, ALU.mult)
                tt("dve", angf[:], angf[:], angw[:], ALU.add, ["angf", "angw"], ["angf"])
                ts("dve", angf[:], angf[:], 3.1415925, ALU.min, ["angf"], ["angf"], -3.1415925, ALU.max)
                act(dst[:], angf[:], AF.Sin, ["angf"], [dstn])

            if first_seg:
                memset("dve", mC[:], 0.0, ["mC"])
                memset("dve", hS[:], 0.0, ["hS"])
                memset("dve", tails[:], 0.0, ["tails"])
                memset("pool", mCbd[:], 0.0, ["mCbd"])

            for l in range(DEPTH):
                k0 = (seq, sg, l)
                dma("sp", gm_b[:], m_norm_g[l:l + 1, :].partition_broadcast(128), "gm_b", [], ["gm_b"])
                dma("sp", gh_b[:], h_norm_g[l:l + 1, :].partition_broadcast(128), "gh_b", [], ["gh_b"])
                for j in range(8):
                    dma("sp", gqk_b[:, j, :], q_norm_g[l:l + 1, :].partition_broadcast(128), "gqk_b", [], ["gqk_b"])
                for j in range(8, 10):
                    dma("sp", gqk_b[:, j, :], k_norm_g[l:l + 1, :].partition_broadcast(128), "gqk_b", [], ["gqk_b"])
                dma("sp", fb_b[:], f_bias[l:l + 1, :].partition_broadcast(128), "fb_b", [], ["fb_b"])
                dma("sp", esink[:], sinks_d[l:l + 1, :].partition_broadcast(128), "esink", [], ["esink"])
                for (ao, oo, n) in A_PIECES:
                    dma("pool", brow_a[0:1, ao:ao + n], b_in[l:l + 1, oo:oo + n], "brow_a", [], ["brow_a"])
                for (bo, oo, n) in B_PIECES:
                    dma("pool", brow_b[0:1, bo:bo + n], b_in[l:l + 1, oo:oo + n], "brow_b", [], ["brow_b"])
                act(esink[:], esink[:], AF.Exp, ["esink"], ["esink"])

                sB, sBn = wslot(k0 + ("B",))
                sA1, sA1n = wslot(k0 + ("A1",))
                sA2, sA2n = wslot(k0 + ("A2",))
                sO, sOn = wslot(k0 + ("O",))

                for g in range(NG):
                    tiles = [g * GT + i for i in range(GT)]
                    norm_tiles(tiles, 0, l, 0)
                    cp("dve", pre[:, :, 0:3], tails[:, l, :, :], ["tails"], ["pre"])
                    for i in range(8):
                        ps, psn = bank_f()
                        for c in range(KC):
                            mm(ps[:, 0:GN], sB[:, c, i * 128:(i + 1) * 128], hT[:, c, 0:GN], c == 0, False,
                               [sBn, "hT"], [psn])
                        mm(ps[:, 0:GN], brow_b[0:1, i * 128:(i + 1) * 128], ones_row[0:1, 0:GN], False, True,
                           ["brow_b", "onesb"], [psn])
                        if i < 4:
                            act(pre[:, i, 3:3 + GN], ps[:, 0:GN], AF.Copy, [psn], ["pre"])
                        elif i < 6:
                            act(hq_s[:, i - 4, 0:GN], ps[:, 0:GN], AF.Silu, [psn], ["hq_s"])
                        else:
                            r = i - 6
                            act(ftmp[:, 0:GN], ps[:, 0:GN], AF.Sigmoid, [psn], ["ftmp"])
                            ts("dve", ftmp[:, 0:GN], ftmp[:, 0:GN], omlb[:, l, r:r + 1], ALU.mult, ["ftmp", "omlb", "lbcol"], ["ftmp"],
                               lbcol[:, l, r:r + 1], ALU.add)
                            ts("dve", key[:, r, 0:GN], ftmp[:, 0:GN], -1.0, ALU.mult, ["ftmp"], ["key"], 1.0, ALU.add)
                            act(ftmp[:, 0:GN], ftmp[:, 0:GN], AF.Ln, ["ftmp"], ["ftmp"])
                            T.op("dve", lambda h, r=r: h.tensor_tensor_scan(
                                out=Gc[:, r, 1:1 + GN], data0=onesb[:, 0:GN], data1=ftmp[:, 0:GN], initial=0.0,
                                op0=ALU.mult, op1=ALU.add), ["ftmp", "onesb"], ["Gc"])
                    for i in range(4):
                        ts("dve", acc[:, 0:GN], pre[:, i, 0:GN], cwcol[:, l, 0, i:i + 1], ALU.mult, ["pre", "cwcol", "cbcol"],
                           ["acc"], cbcol[:, l, i:i + 1], ALU.add)
                        for j in range(1, 4):
                            stt(acc[:, 0:GN], pre[:, i, j:j + GN], cwcol[:, l, j, i:i + 1], acc[:, 0:GN], ALU.mult, ALU.add,
                                ["pre", "cwcol", "acc"], ["acc"])
                        act(qkc[:, i, :], acc[:, 0:GN], AF.Silu, ["acc"], ["qkc"])
                    cp("dve", tails[:, l, :, :], pre[:, :, GN:GN + 3], ["pre"], ["tails"])

                    for lt, t in enumerate(tiles):
                        co = lt * 128
                        xr = "xres%d" % t
                        gblk = sg * T_ + t
                        for pc in range(4):
                            n = (512, 512, 512, 264)[pc]
                            sl, sln = (sA1, sA1n) if pc < 2 else (sA2, sA2n)
                            so_ = (pc % 2) * 512
                            ps, psn = bank_f()
                            for c in range(KC):
                                mm(ps[:, 0:n], hT[:, c, co:co + 128], sl[:, c, so_:so_ + n], c == 0, False,
                                   ["hT", sln], [psn])
                            mm(ps[:, 0:n], ones_row[0:1, 0:128], brow_a[0:1, pc * 512:pc * 512 + n], False, True,
                               ["onesb", "brow_a"], [psn])
                            if pc == 0:
                                cp("dve", Vm[:, :, 0:64], ps[:, 0:256].rearrange("p (h d) -> p h d", d=64), [psn], ["Vm"])
                                act(og[:], ps[:, 256:512], AF.Sigmoid, [psn], ["og"])
                            elif pc == 1:
                                cp("dve", Vh0[0:64, :], ps[0:64, 0:256], [psn], ["Vh0"])
                                cp("dve", Vh1[64:128, :], ps[64:128, 0:256], [psn], ["Vh1"])
                                act(hgs[:], ps[:, 256:512], AF.Silu, [psn], ["hgs"])
                            elif pc == 2:
                                act(sqk[:, 0:8, :], ps[:, 0:512].rearrange("p (h d) -> p h d", d=64), AF.Copy, [psn], ["sqk"])
                            else:
                                cp("dve", sqk[:, 8:10, :], ps[:, 0:128].rearrange("p (h d) -> p h d", d=64), [psn], ["sqk"])
                                cp("dve", Vs[:, t + 1, :, 0:64], ps[:, 128:256].rearrange("p (h d) -> p h d", d=64),
                                   [psn], ["Vs%d" % (t + 1)])
                                cp("dve", gt[:], ps[:, 256:264], [psn], ["gt"])

                        tt("dve", sm[:, 4:8], gt[:, 4:8], fb_b[:], ALU.add, ["gt", "fb_b"], ["sm"])
                        act(sm[:, 4:8], sm[:, 4:8], AF.Exp, ["sm"], ["sm"], scale=-1.0)
                        act(sm[:, 8:12], sm[:, 4:8], AF.Ln, ["sm"], ["sm"], bias=1.0)
                        pg, pgn = bank_f()
                        mm(pg[:, 0:4], tri_f[:], sm[:, 8:12], True, True, ["tri_f", "sm"], [pgn])
                        mm(pg[:, 4:8], ones_f[:], sm[:, 8:12], True, True, ["ones_f", "sm"], [pgn])
                        act(sm[:, 12:16], pg[:, 0:4], AF.Exp, [pgn], ["sm"], scale=-1.0)
                        tt("dve", sm[:, 16:20], gt[:, 0:4], pg[:, 0:4], ALU.add, ["gt", pgn], ["sm"])
                        act(sm[:, 16:20], sm[:, 16:20], AF.Exp, ["sm"], ["sm"], bias=math.log(0.125))
                        for r in range(2):
                            act(sm[0:64, 20 + r:21 + r], pg[0:64, 4 + 2 * r:5 + 2 * r], AF.Exp, [pgn], ["sm"], scale=-1.0)
                            act(sm[64:128, 20 + r:21 + r], pg[64:128, 5 + 2 * r:6 + 2 * r], AF.Exp, [pgn], ["sm"], scale=-1.0)
                        tt("dve", Vt[:], Vm[:], sm[:, 16:20].unsqueeze(2).to_broadcast([128, 4, 65]), ALU.mult,
                           ["Vm", "sm"], ["Vt"])
                        bk, bkn = bank_b()
                        for r in range(2):
                            tr(bk[:, r * 128:(r + 1) * 128], qkc[:, 2 + r, co:co + 128], ident_bf[:], ["qkc", "ident_bf"], [bkn])
                        cp("act", k_tm[:].rearrange("p r d -> p (r d)"), bk[:, 0:256], [bkn], ["k_tm"])
                        for r in range(2):
                            cp("pool", qbd_m[0:64, r, 0:128], qkc[0:64, r, co:co + 128], ["qkc"], ["qbd_m"])
                            cp("pool", qbd_m[64:128, r, 128:256], qkc[64:128, r, co:co + 128], ["qkc"], ["qbd_m"])
                        ps, psn = bank_f()
                        for r in range(2):
                            mm(ps[:, r * 256:(r + 1) * 256], qkc[:, 2 + r, co:co + 128], qbd_m[:, r, :], True, True,
                               ["qkc", "qbd_m"], [psn])
                        tt("dve", Wt[:], ps[:, :], mask4[:].rearrange("p a b -> p (a b)"), ALU.mult, [psn, "mask4"], ["Wt"])
                        po, pon = bank_f()
                        for r in range(2):
                            mm(po[:, r * 130:(r + 1) * 130], qkc[:, r, co:co + 128], mCbd[:, l, r, :], True, False,
                               ["qkc", "mCbd"], [pon])
                            for hb in range(2):
                                h_ = 2 * r + hb
                                mm(po[:, h_ * 65:(h_ + 1) * 65], Wt[:, h_ * 128:(h_ + 1) * 128], Vt[:, h_, :], False,
                                   hb == 1, ["Wt", "Vt"], [pon])
                        pu, pun = bank_f()
                        for r in range(2):
                            mm(pu[:, r * 130:(r + 1) * 130], k_tm[:, r, :],
                               Vt[:, 2 * r:2 * r + 2, :].rearrange("p h d -> p (h d)"), True, True, ["k_tm", "Vt"], [pun])
                        tt("dve", tmpC[:], mC[:, l, :, :], sm[:, 20:22].unsqueeze(2).to_broadcast([128, 2, 65]), ALU.mult,
                           ["mC", "sm"], ["tmpC"])
                        for r in range(2):
                            for hb in range(2):
                                prt = slice(hb * 64, (hb + 1) * 64)
                                stt(mC[prt, l, r, :], pu[prt, r * 130 + hb * 65:r * 130 + (hb + 1) * 65],
                                    sm[prt, 20 + r:21 + r], tmpC[prt, r, :], ALU.mult, ALU.add,
                                    [pun, "sm", "tmpC"], ["mC"])
                                cp("pool", mCbd[prt, l, r, hb * 65:(hb + 1) * 65], mC[prt, l, r, :], ["mC"], ["mCbd"])
                        tt("dve", hr[:], po[:, 0:260].rearrange("p (h d) -> p h d", d=65),
                           sm[:, 12:16].unsqueeze(2).to_broadcast([128, 4, 65]), ALU.mult, [pon, "sm"], ["hr"])
                        act(sm[:, 24:28], hr[:, :, 64], AF.Abs, ["hr"], ["sm"])
                        ts("dve", sm[:, 24:28], sm[:, 24:28], 1.0, ALU.max, ["sm"], ["sm"])
                        recip(sm[:, 24:28], sm[:, 24:28], ["sm"], ["sm"])
                        t4 = t256[:].rearrange("p (h d) -> p h d", d=64)
                        u4 = u256[:].rearrange("p (h d) -> p h d", d=64)
                        tt("dve", t4, hr[:, :, 0:64], sm[:, 24:28].unsqueeze(2).to_broadcast([128, 4, 64]), ALU.mult,
                           ["hr", "sm"], ["t256"])
                        tt("dve", u4, t4, t4, ALU.mult, ["t256"], ["u256"])
                        reduce_add(sm[:, 28:32], u4, ["u256"], ["sm"])
                        act(sm[:, 28:32], sm[:, 28:32], AF.Sqrt, ["sm"], ["sm"], bias=EPS, scale=1.0 / 64)
                        recip(sm[:, 28:32], sm[:, 28:32], ["sm"], ["sm"])
                        tt("dve", t4, t4, sm[:, 28:32].unsqueeze(2).to_broadcast([128, 4, 64]), ALU.mult,
                           ["t256", "sm"], ["t256"])
                        tt("dve", t256[:], t256[:], gm_b[:], ALU.mult, ["t256", "gm_b"], ["t256"])
                        tt("dve", mix[:, 0:256], t256[:], og[:], ALU.mult, ["t256", "og"], ["mix"])

                        c_prev = co
                        gmid = Gc[:, :, co + 32:co + 129:64]
                        gend = Gc[:, :, co + 64:co + 129:64]
                        gprv = Gc[:, :, co:co + 65:64]
                        tt("dve", hgE[:, 0, :, :], gmid, gprv, ALU.subtract, ["Gc"], ["hgE"])
                        tt("dve", hgE[:, 1, :, :], gend, gmid, ALU.subtract, ["Gc"], ["hgE"])
                        tt("dve", hgE[:, 2, :, :], gend, gprv, ALU.subtract, ["Gc"], ["hgE"])
                        act(hgE[:], hgE[:], AF.Exp, ["hgE"], ["hgE"])
                        for r in range(2):
                            ts("dve", sm2[:, 2 * r:2 * r + 2], Gc[:, r, co + 32:co + 129:64], -1.0, ALU.mult, ["Gc"], ["sm2"])
                        for r in range(2):
                            for cc in range(2):
                                cs = co + cc * 64
                                midc = 1 + cs + 31
                                act(E1[:], Gc[:, r, 1 + cs:1 + cs + 64], AF.Exp, ["Gc", "sm2"], ["E1"],
                                    bias=sm2[:, 2 * r + cc:2 * r + cc + 1])
                                tt("dve", qh[:, r, cc * 64:(cc + 1) * 64], hq_s[:, r, cs:cs + 64], E1[:], ALU.mult,
                                   ["hq_s", "E1"], ["qh"])
                                act(E2[:], Gc[:, r, 1 + cs:1 + cs + 64], AF.Exp, ["Gc"], ["E2"],
                                    bias=Gc[:, r, midc:midc + 1], scale=-1.0)
                                tt("dve", kh[:, r, cc * 64:(cc + 1) * 64], key[:, r, cs:cs + 64], E2[:], ALU.mult,
                                   ["key", "E2"], ["kh"])
                                for hb in range(2):
                                    prt = slice(hb * 64, (hb + 1) * 64)
                                    cp("pool", qbd_h[prt, r, cc, hb * 64:(hb + 1) * 64], qh[prt, r, cc * 64:(cc + 1) * 64],
                                       ["qh"], ["qbd_h"])
                        bk, bkn = bank_b()
                        for r in range(2):
                            tr(bk[:, r * 128:(r + 1) * 128], kh[:, r, :], ident_bf[:], ["kh", "ident_bf"], [bkn])
                        cp("act", kh_tm[:].rearrange("p r d -> p (r d)"), bk[:, 0:256], [bkn], ["kh_tm"])
                        ps, psn = bank_f()
                        for cc in range(2):
                            for r in range(2):
                                mm(ps[cc * 64:(cc + 1) * 64, r * 128:(r + 1) * 128], kh[:, r, cc * 64:(cc + 1) * 64],
                                   qbd_h[:, r, cc, :], True, True, ["kh", "qbd_h"], [psn])
                        tt("dve", At[:], ps[:, 0:256], mask64[:].rearrange("p a b -> p (a b)"), ALU.mult,
                           [psn, "mask64"], ["At"])
                        po, pon = bank_f()
                        Vhs = (Vh0, Vh1)
                        Vhn = ("Vh0", "Vh1")
                        for cc in range(2):
                            cpr = slice(cc * 64, (cc + 1) * 64)
                            for hb in range(2):
                                prt = slice(hb * 64, (hb + 1) * 64)
                                tt("dve", hSbd[prt, :, hb * 64:(hb + 1) * 64], hS[prt, l, :, :],
                                   hgE[prt, 0, :, cc:cc + 1].to_broadcast([64, 2, 64]), ALU.mult, ["hS", "hgE"], ["hSbd"])
                            for r in range(2):
                                mm(po[cpr, r * 128:(r + 1) * 128], qh[:, r, cc * 64:(cc + 1) * 64], hSbd[:, r, :], True, False,
                                   ["qh", "hSbd"], [pon])
                                for hb in range(2):
                                    h_ = 2 * r + hb
                                    mm(po[cpr, h_ * 64:(h_ + 1) * 64], At[:, h_ * 64:(h_ + 1) * 64],
                                       Vhs[cc][:, h_ * 64:(h_ + 1) * 64], False, hb == 1, ["At", Vhn[cc]], [pon])
                            pu, pun = bank_f()
                            for r in range(2):
                                mm(pu[:, r * 128:(r + 1) * 128], kh_tm[:, r, :], Vhs[cc][:, r * 128:(r + 1) * 128], True, True,
                                   ["kh_tm", Vhn[cc]], [pun])
                            tt("dve", tmpS[:], hS[:, l, :, :], hgE[:, 2, :, cc:cc + 1].to_broadcast([128, 2, 64]), ALU.mult,
                               ["hS", "hgE"], ["tmpS"])
                            for r in range(2):
                                for hb in range(2):
                                    prt = slice(hb * 64, (hb + 1) * 64)
                                    stt(hS[prt, l, r, :], pu[prt, r * 128 + hb * 64:r * 128 + (hb + 1) * 64],
                                        hgE[prt, 1, r, cc:cc + 1], tmpS[prt, r, :], ALU.mult, ALU.add,
                                        [pun, "hgE", "tmpS"], ["hS"])
                        cp("act", t256[:], po[:, 0:256], [pon], ["t256"])
                        tt("dve", u256[:], t256[:], t256[:], ALU.mult, ["t256"], ["u256"])
                        reduce_add(sm2[:, 8:12], u256[:].rearrange("p (h d) -> p h d", d=64), ["u256"], ["sm2"])
                        act(sm2[:, 8:12], sm2[:, 8:12], AF.Sqrt, ["sm2"], ["sm2"], bias=EPS, scale=1.0 / 64)
                        recip(sm2[:, 8:12], sm2[:, 8:12], ["sm2"], ["sm2"])
                        tt("dve", t256[:].rearrange("p (h d) -> p h d", d=64), t256[:].rearrange("p (h d) -> p h d", d=64),
                           sm2[:, 8:12].unsqueeze(2).to_broadcast([128, 4, 64]), ALU.mult, ["t256", "sm2"], ["t256"])
                        tt("dve", t256[:], t256[:], gh_b[:], ALU.mult, ["t256", "gh_b"], ["t256"])
                        tt("dve", mix[:, 256:512], t256[:], hgs[:], ALU.mult, ["t256", "hgs"], ["mix"])

                        tt("dve", sqt[:], sqk[:], sqk[:], ALU.mult, ["sqk"], ["sqt"])
                        reduce_add(sm2[:, 16:26], sqt[:], ["sqt"], ["sm2"])
                        act(sm2[:, 16:26], sm2[:, 16:26], AF.Sqrt, ["sm2"], ["sm2"], bias=EPS, scale=1.0 / 64)
                        recip(sm2[:, 16:26], sm2[:, 16:26], ["sm2"], ["sm2"])
                        tt("dve", sqk[:], sqk[:], sm2[:, 16:26].unsqueeze(2).to_broadcast([128, 10, 64]), ALU.mult,
                           ["sqk", "sm2"], ["sqk"])
                        tt("dve", sqk[:], sqk[:], gqk_b[:], ALU.mult, ["sqk", "gqk_b"], ["sqk"])
                        cosb = rope_c[:, t, :].unsqueeze(1).to_broadcast([128, 10, 8])
                        sinb = rope_s[:, t, :].unsqueeze(1).to_broadcast([128, 10, 8])
                        tt("dve", rtmp[:, 0], sqk[:, :, 0:8], cosb, ALU.mult, ["sqk", "rope_c"], ["rtmp"])
                        tt("dve", rtmp[:, 1], sqk[:, :, 8:16], sinb, ALU.mult, ["sqk", "rope_s"], ["rtmp"])
                        tt("dve", rtmp[:, 2], sqk[:, :, 8:16], cosb, ALU.mult, ["sqk", "rope_c"], ["rtmp"])
                        tt("dve", rtmp[:, 3], sqk[:, :, 0:8], sinb, ALU.mult, ["sqk", "rope_s"], ["rtmp"])
                        cp("act", qkr[:], sqk[:], ["sqk"], ["qkr"])
                        tt("dve", qkr[:, :, 0:8], rtmp[:, 0], rtmp[:, 1], ALU.subtract, ["rtmp", "qkr"], ["qkr"])
                        tt("dve", qkr[:, :, 8:16], rtmp[:, 2], rtmp[:, 3], ALU.add, ["rtmp", "qkr"], ["qkr"])
                        bk, bkn = bank_b()
                        qkr2 = qkr[:].rearrange("p h d -> p (h d)")
                        for j in range(5):
                            tr(bk[:, j * 128:(j + 1) * 128], qkr2[:, j * 128:(j + 1) * 128], ident_bf[:], ["qkr", "ident_bf"], [bkn])
                        cp("act", QT0[0:64, :], bk[0:64, 0:512], [bkn], ["QT0"])
                        cp("act", QT1[64:128, :], bk[64:128, 0:512], [bkn], ["QT1"])
                        cp("dve", KT[:, (t + 1) * 128:(t + 2) * 128], bk[:, 512:640], [bkn], ["KT%d" % (t + 1)])
                        QTs = ((QT0, "QT0"), (QT1, "QT1"))
                        kvsrc = []
                        if gblk > 0:
                            if t == 0:
                                kvsrc.append((KTcar[:, l, :], "KTcar", Vscar[:, l, :, :], "Vscar", nm_prev, "nm_prev"))
                            else:
                                kvsrc.append((KT[:, t * 128:(t + 1) * 128], "KT%d" % t, Vs[:, t, :, :], "Vs%d" % t,
                                              nm_prev, "nm_prev"))
                        kvsrc.append((KT[:, (t + 1) * 128:(t + 2) * 128], "KT%d" % (t + 1), Vs[:, t + 1, :, :],
                                      "Vs%d" % (t + 1), nm_cur, "nm_cur"))
                        pi = 0
                        for hk in range(2):
                            pts = []
                            for (kap, kn, vap, vn, nmk, nmn) in kvsrc:
                                ps, psn = bank_f()
                                mm(ps[:, :], kap, QTs[hk][0][:, :], True, False, [kn, QTs[hk][1]], [psn])
                                mm(ps[:, :], ident_bf[:], nmk[:].rearrange("p a b -> p (a b)"), False, True,
                                   ["ident_bf", nmn], [psn])
                                ptile_ = Pt[pi % 2]
                                ptn = "Pt%d" % (pi % 2)
                                pi += 1
                                act(ptile_[:], ps[:, :], AF.Exp, [psn], [ptn], scale=0.125)
                                pts.append((ptile_, ptn, vap, vn))
                            po, pon = bank_f()
                            for g_ in range(4):
                                for mi_, (ptile_, ptn, vap, vn) in enumerate(pts):
                                    mm(po[:, g_ * 65:(g_ + 1) * 65], ptile_[:, g_ * 128:(g_ + 1) * 128], vap[:, hk, :],
                                       mi_ == 0, mi_ == len(pts) - 1, [ptn, vn], [pon])
                            cp("act", so[:], po[:, 0:260].rearrange("p (h d) -> p h d", d=65), [pon], ["hr"])
                            tt("dve", sm2[:, 32:36], so[:, :, 64], esink[:, hk * 4:(hk + 1) * 4], ALU.add, ["hr", "esink"], ["sm2"])
                            recip(sm2[:, 32:36], sm2[:, 32:36], ["sm2"], ["sm2"])
                            tt("dve", mix[:, 512 + hk * 256:512 + (hk + 1) * 256].rearrange("p (h d) -> p h d", d=64),
                               so[:, :, 0:64], sm2[:, 32:36].unsqueeze(2).to_broadcast([128, 4, 64]), ALU.mult,
                               ["hr", "sm2"], ["mix"])
                        if t == T_ - 1:
                            cp("pool", KTcar[:, l, :], KT[:, T_ * 128:(T_ + 1) * 128], ["KT%d" % T_], ["KTcar"])
                            cp("pool", Vscar[:, l, :, :], Vs[:, T_, :, :], ["Vs%d" % T_], ["Vscar"])

                        bk, bkn = bank_b()
                        for c in range(KC):
                            tr(bk[:, c * 128:(c + 1) * 128], mix[:, c * 128:(c + 1) * 128], ident_bf[:], ["mix", "ident_bf"], [bkn])
                        cp("act", xn[:], bk[:, :], [bkn], ["xn"])
                        for nh in range(2):
                            ps, psn = bank_f()
                            for c in range(KC):
                                mm(ps[:, :], mixT[:, c, :], sO[:, c, nh * 512:(nh + 1) * 512], c == 0, c == KC - 1,
                                   ["xn", sOn], [psn])
                            tt("dve", xres[:, t, nh * 512:(nh + 1) * 512], xres[:, t, nh * 512:(nh + 1) * 512], ps[:, :],
                               ALU.add, [xr, psn], [xr])
                release(k0 + ("B",))
                release(k0 + ("A1",))
                release(k0 + ("A2",))
                release(k0 + ("O",))

                norm_tiles(list(range(T_)), 1, l, 0)
                for j in range(4):
                    sU, sUn = wslot(k0 + ("U%d" % j,))
                    sD, sDn = wslot(k0 + ("D%d" % j,))
                    for gi in range(T_ // MG):
                        gc0 = gi * MGN
                        for i in range(KC):
                            ps, psn = bank_f()
                            for c in range(KC):
                                mm(ps[:, 0:MGN], sU[:, c, i * 128:(i + 1) * 128], hT[:, c, gc0:gc0 + MGN], c == 0, c == KC - 1,
                                   [sUn, "hT"], [psn])
                            act(sqv[:, 0:MGN], ps[:, 0:MGN], AF.Square, [psn], ["acc"])
                            stt(aT[:, i, :], ps[:, 0:MGN], 0.0, sqv[:, 0:MGN], ALU.is_gt, ALU.mult, [psn, "acc"], ["pre"])
                        for lt in range(MG):
                            t = gi * MG + lt
                            xr = "xres%d" % t
                            for nh in range(2):
                                ps, psn = bank_f()
                                for i in range(KC):
                                    mm(ps[:, :], aT[:, i, lt * 128:(lt + 1) * 128], sD[:, i, nh * 512:(nh + 1) * 512],
                                       i == 0, i == KC - 1, ["pre", sDn], [psn])
                                tt("dve", xres[:, t, nh * 512:(nh + 1) * 512], xres[:, t, nh * 512:(nh + 1) * 512], ps[:, :],
                                   ALU.add, [xr, psn], [xr])
                    release(k0 + ("U%d" % j,))
                    release(k0 + ("D%d" % j,))

                norm_tiles(list(range(T_)), 2, l, 0)
                dma("sp", gpost_b, post_g[l:l + 1, :].partition_broadcast(128), "gpost_b", [], ["key"])
                sG, sGn = wslot(k0 + ("G",))
                sP, sPn = wslot(k0 + ("P",))
                for t in range(T_):
                    xr = "xres%d" % t
                    c0 = t * 128
                    pti = ptile[t % 2]
                    ptn = ("t256", "u256")[t % 2]
                    dma("sp", pti[:], p_d[l, tok0 + c0:tok0 + c0 + 128, :], ptn, [], [ptn])
                    cp("pool", pbf[:], pti[:], [ptn], ["pbf"])
                    bk, bkn = bank_b()
                    for c in range(2):
                        tr(bk[:, c * 128:(c + 1) * 128], pbf[:, c * 128:(c + 1) * 128], ident_bf[:], ["pbf", "ident_bf"], [bkn])
                    cp("act", pT[:].rearrange("p c t -> p (c t)"), bk[:, 0:256], [bkn], ["pT"])
                    pe_banks = []
                    for nh in range(2):
                        ps, psn = bank_f()
                        for c in range(KC):
                            mm(ps[:, :], hT[:, c, c0:c0 + 128], sG[:, c, nh * 512:(nh + 1) * 512], c == 0, c == KC - 1,
                               ["hT", sGn], [psn])
                        act(gate[:, nh * 512:(nh + 1) * 512], ps[:, :], AF.Sigmoid, [psn], ["hq_s"])
                    for nh in range(2):
                        ps, psn = bank_f()
                        for c in range(2):
                            mm(ps[:, :], pT[:, c, :], sP[:, c, nh * 512:(nh + 1) * 512], c == 0, c == 1, ["pT", sPn], [psn])
                        pe_banks.append((ps, psn))
                        act(xn[:, 0:512], ps[:, :], AF.Square, [psn], ["xn", "sm2"], accum=sm2[:, 40 + nh:41 + nh])
                    tt("dve", sm2[:, 42:43], sm2[:, 40:41], sm2[:, 41:42], ALU.add, ["sm2"], ["sm2"])
                    act(sm2[:, 42:43], sm2[:, 42:43], AF.Sqrt, ["sm2"], ["sm2"], bias=EPS, scale=1.0 / D)
                    recip(sm2[:, 42:43], sm2[:, 42:43], ["sm2"], ["sm2"])
                    for nh in range(2):
                        ps, psn = pe_banks[nh]
                        stt(etmp, ps[:, :], sm2[:, 42:43], gpost_b[:, nh * 512:(nh + 1) * 512], ALU.mult, ALU.mult,
                            [psn, "sm2", "key"], ["ftmp"])
                        tt("dve", etmp, etmp, gate[:, nh * 512:(nh + 1) * 512], ALU.mult, ["ftmp", "hq_s"], ["ftmp"])
                        tt("dve", xres[:, t, nh * 512:(nh + 1) * 512], xres[:, t, nh * 512:(nh + 1) * 512], etmp, ALU.add,
                           [xr, "ftmp"], [xr])
                release(k0 + ("G",))
                release(k0 + ("P",))

            for t in range(T_):
                dma("sp", out_d[tok0 + t * 128: tok0 + (t + 1) * 128, :], xres[:, t, :], "st%d" % t,
                    ["xres%d" % t], ["out%d" % t])
    T.final_wait("sp", ["out%d" % t for t in range(T_)])
    T.emit(nc, st, cfg.get("EPOCH", 16000))
    st.close()
    return nc


_PARAM_NAMES = ["in_norm_g", "w_in", "b_in", "mlstm_f_bias", "mlstm_conv_w", "mlstm_conv_b", "mlstm_norm_g",
                "hgrn_lb_logits", "hgrn_norm_g", "swa_q_norm_g", "swa_k_norm_g", "swa_sinks", "w_out", "mlp_norm_g",
                "w_up", "w_down", "ple_norm_g", "w_ple_gate", "w_ple_proj", "ple_post_norm_g"]


def make_in_maps(inputs, n_cores):
    x = np.asarray(inputs["x"])
    p = np.asarray(inputs["p"])
    pos = np.asarray(inputs["positions"])
    B, S, _ = x.shape
    depth = p.shape[0]
    nseq = B // n_cores
    params = {k: np.ascontiguousarray(np.asarray(inputs[k], dtype=np.float32)) for k in _PARAM_NAMES}
    maps = []
    for c in range(n_cores):
        sl = slice(c * nseq, (c + 1) * nseq)
        m = dict(params)
        m["x"] = np.ascontiguousarray(x[sl].reshape(nseq * S, D))
        m["p"] = np.ascontiguousarray(p[:, sl].reshape(depth, nseq * S, PLE))
        m["positions"] = np.ascontiguousarray(pos[sl].reshape(nseq * S).astype(np.int32))
        maps.append(m)
    return maps, nseq


N_LAUNCH = 4


def kernel(**inputs):
    x = np.asarray(inputs["x"])
    B, S, _ = x.shape
    depth = np.asarray(inputs["p"]).shape[0]
    nseq_total = B // N_CORES
    per = nseq_total // N_LAUNCH
    out = np.zeros((B, S, D), dtype=np.float32)
    cfg = dict(S=S, SEG=min(1024, S), GT=4, DEPTH=depth, NSEQ=per)
    nc = build_program(cfg)
    xs = x.reshape(N_CORES, nseq_total, S, D)
    ps = np.asarray(inputs["p"]).reshape(depth, N_CORES, nseq_total, S, PLE)
    pos = np.asarray(inputs["positions"]).reshape(N_CORES, nseq_total, S)
    for li in range(N_LAUNCH):
        sl = slice(li * per, (li + 1) * per)
        sub = dict(inputs)
        sub["x"] = xs[:, sl].reshape(N_CORES * per, S, D)
        sub["p"] = ps[:, :, sl].reshape(depth, N_CORES * per, S, PLE)
        sub["positions"] = pos[:, sl].reshape(N_CORES * per, S)
        maps, nseq = make_in_maps(sub, N_CORES)
        res = run_bass_kernel_spmd(nc, maps, core_ids=list(range(N_CORES)))
        o = out.reshape(N_CORES, nseq_total, S, D)
        for c, r in enumerate(res.results):
            o[c, sl] = np.asarray(r["out"]).reshape(per, S, D)
    return out
```

```python
import contextlib
import math

import numpy as np
import concourse.bass as bass
import concourse.mybir as mybir
from concourse.bass_utils import run_bass_kernel_spmd

F32 = mybir.dt.float32
BF16 = mybir.dt.bfloat16
I32 = mybir.dt.int32
AF = mybir.ActivationFunctionType
ALU = mybir.AluOpType
AX = mybir.AxisListType

N_CORES = 8
D = 1024
KC = 8
DFF = 4096
PLE = 256
IN_W = 2824
EPS = 1e-6
ROPE_THETA = 500000.0
NEG = -30000.0
TWO_PI = 2.0 * math.pi

O_MQ, O_MK, O_MV, O_MO, O_MI, O_MF = 0, 256, 512, 768, 1024, 1028
O_HQ, O_HF, O_HI, O_HG = 1032, 1288, 1544, 1800
O_SQ, O_SK, O_SV = 2056, 2568, 2696
QPERM = [0, 4, 1, 5, 2, 6, 3, 7]
A_PIECES = [(0, O_MV, 256), (256, O_MO, 256), (512, O_HI, 256), (768, O_HG, 256)]
A_PIECES += [(1024 + j * 64, O_SQ + QPERM[j] * 64, 64) for j in range(8)]
A_PIECES += [(1536, O_SK, 128), (1664, O_SV, 128), (1792, O_MI, 4), (1796, O_MF, 4)]
A_W = 1800
B_PIECES = [(0, O_MQ, 256), (256, O_MK, 256), (512, O_HQ, 256), (768, O_HF, 256)]
B_W = 1024

ENGS = ("pe", "act", "dve", "pool", "sp")


class Tracker:
    def __init__(self):
        self.streams = {e: [] for e in ENGS}
        self.count = {e: 0 for e in ENGS}
        self.waited = {e: {} for e in ENGS}
        self.last_w = {}
        self.readers = {}
        self.dma_cum = {}

    def _need(self, eng, deps, pe_skip=True):
        best = {}
        for d in deps:
            if d is None:
                continue
            kind, key, n = d
            if kind == "e" and key == eng and eng == "pe" and pe_skip:
                continue
            if kind == "d":
                n = self.dma_cum[key]
            k = (kind, key)
            if n > best.get(k, 0):
                best[k] = n
        for k, n in best.items():
            if n > self.waited[eng].get(k, 0):
                self.waited[eng][k] = n
                self.streams[eng].append(("wait", k, n))

    def _deps_for(self, reads, writes):
        deps = []
        for r in reads:
            deps.append(self.last_w.get(r))
        for w in writes:
            deps.append(self.last_w.get(w))
            deps.extend(self.readers.get(w, ()))
        return deps

    def _commit(self, me, reads, writes):
        for r in reads:
            self.readers.setdefault(r, []).append(me)
        for w in writes:
            self.last_w[w] = me
            self.readers[w] = []

    def op(self, eng, fn, reads=(), writes=()):
        self._need(eng, self._deps_for(reads, writes))
        self.count[eng] += 1
        me = ("e", eng, self.count[eng])
        self.streams[eng].append(("ins", fn, None))
        self._commit(me, reads, writes)

    def dma(self, q, fn, slot, reads=(), writes=()):
        self._need(q, self._deps_for(reads, writes))
        self.dma_cum[slot] = self.dma_cum.get(slot, 0) + 16
        me = ("d", slot, self.dma_cum[slot])
        self.streams[q].append(("dma", fn, slot))
        self._commit(me, reads, writes)

    def final_wait(self, eng, resources):
        deps = []
        for r in resources:
            deps.append(self.last_w.get(r))
            deps.extend(self.readers.get(r, ()))
        self._need(eng, deps, pe_skip=False)

    def emit(self, nc, stack, epoch=32000):
        sems = {}
        for e in ENGS:
            for k in range(self.count[e] // epoch + 1):
                sems[("e", e, k)] = stack.enter_context(nc.semaphore("s_%s%d" % (e, k)))
        for i, slot in enumerate(self.dma_cum):
            sems[("d", slot)] = stack.enter_context(nc.semaphore("d%d" % i))
        block = stack.enter_context(nc.Block())
        hmap = {"pe": block.tensor, "act": block.scalar, "dve": block.vector,
                "pool": block.gpsimd, "sp": block.sync}

        def make(e):
            def body(h):
                n_ins = 0
                for kind, a, b in self.streams[e]:
                    if kind == "wait":
                        if a[0] == "e":
                            h.wait_ge(sems[("e", a[1], (b - 1) // epoch)], (b - 1) % epoch + 1)
                        else:
                            h.wait_ge(sems[a], b)
                    elif kind == "ins":
                        a(h).then_inc(sems[("e", e, n_ins // epoch)], 1)
                        n_ins += 1
                    else:
                        a(h).then_inc(sems[("d", b)], 16)
            return body

        for e in ENGS:
            if self.streams[e]:
                hmap[e](make(e))


def build_program(cfg):
    S = cfg["S"]
    SEG = cfg["SEG"]
    GT = cfg["GT"]
    DEPTH = cfg["DEPTH"]
    NSEQ = cfg["NSEQ"]
    T_ = SEG // 128
    NSEGS = S // SEG
    NG = T_ // GT
    GN = GT * 128
    MG = min(4, T_)
    MGN = MG * 128
    NTOK = NSEQ * S

    nc = bass.Bass("TRN2", target_bir_lowering=False)
    dram = {}

    def din(name, shape, dt=F32):
        dram[name] = nc.dram_tensor(name, list(shape), dt, kind="ExternalInput").ap()
        return dram[name]

    x_d = din("x", [NTOK, D])
    p_d = din("p", [DEPTH, NTOK, PLE])
    pos_d = din("positions", [NTOK], I32)
    in_norm_g = din("in_norm_g", [DEPTH, D])
    w_in = din("w_in", [DEPTH, D, IN_W])
    b_in = din("b_in", [DEPTH, IN_W])
    f_bias = din("mlstm_f_bias", [DEPTH, 4])
    conv_w = din("mlstm_conv_w", [DEPTH, 4, 512])
    conv_b = din("mlstm_conv_b", [DEPTH, 512])
    m_norm_g = din("mlstm_norm_g", [DEPTH, 256])
    lb_logits = din("hgrn_lb_logits", [DEPTH, 256])
    h_norm_g = din("hgrn_norm_g", [DEPTH, 256])
    q_norm_g = din("swa_q_norm_g", [DEPTH, 64])
    k_norm_g = din("swa_k_norm_g", [DEPTH, 64])
    sinks_d = din("swa_sinks", [DEPTH, 8])
    w_out = din("w_out", [DEPTH, D, D])
    mlp_norm_g = din("mlp_norm_g", [DEPTH, D])
    w_up = din("w_up", [DEPTH, D, DFF])
    w_down = din("w_down", [DEPTH, DFF, D])
    ple_norm_g = din("ple_norm_g", [DEPTH, D])
    w_gate = din("w_ple_gate", [DEPTH, D, D])
    w_proj = din("w_ple_proj", [DEPTH, PLE, D])
    post_g = din("ple_post_norm_g", [DEPTH, D])
    out_d = nc.dram_tensor("out", [NTOK, D], F32, kind="ExternalOutput").ap()

    T = Tracker()
    st = contextlib.ExitStack()

    NBLK = 14
    wblk = nc.dram_tensor("wblk", [DEPTH, NBLK, 128, KC * 1024], BF16, kind="Internal").ap()

    def sb(name, shape, dt=F32):
        return st.enter_context(nc.sbuf_tensor(name, list(shape), dt))

    xres = sb("xres", [128, T_, D])
    slots = [sb("slot%d" % i, [128, KC, 1024], BF16) for i in range(4)]
    hT = sb("hT", [128, KC, SEG], BF16)
    xn = sb("xn", [128, D], BF16)
    PREW = max(GN, MGN) + 3
    pre = sb("pre", [128, 4, PREW])
    GNA = max(GN, 512)
    acc = sb("acc", [128, GNA])
    qkc = sb("qkc", [128, 4, GN], BF16)
    hq_s = sb("hq_s", [128, 2, GNA])
    Gc = sb("Gc", [128, 2, GN + 1])
    key = sb("key", [128, 2, GNA])
    ftmp = sb("ftmp", [128, GNA])
    tails = sb("tails", [128, DEPTH, 4, 3])
    og = sb("og", [128, 256])
    hgs = sb("hgs", [128, 256])
    Vm = sb("Vm", [128, 4, 65], BF16)
    Vt = sb("Vt", [128, 4, 65], BF16)
    Vh0 = sb("Vh0", [128, 256], BF16)
    Vh1 = sb("Vh1", [128, 256], BF16)
    sq2 = sb("sq2", [128, 2, 10, 64])
    sqk = sq2[:, 0]
    sqt = sq2[:, 1]
    gt = sb("gt", [128, 8])
    grep = sq2[:].rearrange("p a h d -> p (a h d)")[:, 0:KC * 128].rearrange("p (c t) -> p c t", t=128)
    Vs = sb("Vs", [128, T_ + 1, 2, 65], BF16)
    KT = sb("KT", [128, (T_ + 1) * 128], BF16)
    KTcar = sb("KTcar", [128, DEPTH, 128], BF16)
    Vscar = sb("Vscar", [128, DEPTH, 2, 65], BF16)
    QT0 = sb("QT0", [128, 512], BF16)
    QT1 = sb("QT1", [128, 512], BF16)
    Pt = [sb("Pt%d" % i, [128, 512], BF16) for i in range(2)]
    Wt = sb("Wt", [128, 512], BF16)
    At = sb("At", [128, 256], BF16)
    k_tm = sb("k_tm", [128, 2, 128], BF16)
    kh_tm = sb("kh_tm", [128, 2, 128], BF16)
    qbd_m = sb("qbd_m", [128, 2, 256], BF16)
    qh = sb("qh", [128, 2, 128], BF16)
    kh = sb("kh", [128, 2, 128], BF16)
    qbd_h = sb("qbd_h", [128, 2, 2, 128], BF16)
    E1 = sb("E1", [128, 64])
    E2 = sb("E2", [128, 64])
    mix = sb("mix", [128, D], BF16)
    mixT = xn[:].rearrange("p (c t) -> p c t", t=128)
    mC = sb("mC", [128, DEPTH, 2, 65])
    mCbd = sb("mCbd", [128, DEPTH, 2, 130], BF16)
    tmpC = sb("tmpC", [128, 2, 65])
    hS = sb("hS", [128, DEPTH, 2, 64])
    hSbd = sb("hSbd", [128, 2, 128], BF16)
    tmpS = sb("tmpS", [128, 2, 64])
    hr = sb("hr", [128, 4, 65])
    t256 = sb("t256", [128, 256])
    u256 = sb("u256", [128, 256])
    so = sb("so", [128, 4, 65])
    smm = sb("smm", [128, 64])
    smh = sb("smh", [128, 64])
    sms = sb("sms", [128, 64])
    t256h = sb("t256h", [128, 256])
    u256h = sb("u256h", [128, 256])
    sm = sb("sm", [128, 64])
    sm2 = sb("sm2", [128, 64])
    hgE = sb("hgE", [128, 3, 2, 2])
    rope_c = sb("rope_c", [128, T_, 8])
    rope_s = sb("rope_s", [128, T_, 8])
    rtmp = sb("rtmp", [128, 4, 10, 8])
    qkr = sb("qkr", [128, 10, 64], BF16)
    ident_bf = sb("ident_bf", [128, 128], BF16)
    ident_f = sb("ident_f", [128, 128])
    tri_f = sb("tri_f", [128, 128])
    ones_f = sb("ones_f", [128, 128])
    onesb = sb("onesb", [128, 512], BF16)
    mask4 = sb("mask4", [128, 4, 128], BF16)
    mask64 = sb("mask64", [128, 4, 64], BF16)
    nm_cur = sb("nm_cur", [128, 4, 128], BF16)
    nm_prev = sb("nm_prev", [128, 4, 128], BF16)
    ones_row = onesb
    brow_a = sb("brow_a", [1, A_W], BF16)
    brow_b = sb("brow_b", [1, B_W], BF16)
    stg1 = t256
    stg2 = u256
    gcols = sb("gcols", [128, 3, DEPTH, KC])
    cwcol = sb("cwcol", [128, DEPTH, 4, 4])
    cbcol = sb("cbcol", [128, DEPTH, 4])
    lbcol = sb("lbcol", [128, DEPTH, 2])
    lbe = sb("lbe", [128, DEPTH, 2])
    omlb = sb("omlb", [128, DEPTH, 2])
    gm_b = sb("gm_b", [128, 256])
    gh_b = sb("gh_b", [128, 256])
    gqk_b = sb("gqk_b", [128, 10, 64])
    fb_b = sb("fb_b", [128, 4])
    esink = sb("esink", [128, 8])
    aT = pre[:].rearrange("p a b -> p (a b)").bitcast(BF16)[:, 0:KC * MGN].rearrange("p (c n) -> p c n", n=MGN)
    sqv = acc
    ptile = [t256, u256]
    gpost_b = key[:, :, 0:512].rearrange("p r n -> p (r n)")
    gate = hq_s[:, :, 0:512].rearrange("p r n -> p (r n)")
    etmp = ftmp[:, 0:512]
    zsrc_t = acc
    pbf = sb("pbf", [128, PLE], BF16)
    pT = sb("pT", [128, 2, 128], BF16)
    post = sb("post", [128, T_], I32)
    posf = sb("posf", [128, T_])
    angk = sb("angk", [128, T_, 8])
    angi = sb("angi", [128, T_, 8], I32)
    angf = sb("angf", [128, T_, 8])
    angm = sb("angm", [128, T_, 8])
    angw = sb("angw", [128, T_, 8])

    pf = [st.enter_context(nc.psum_tensor("pf%d" % i, [128, 512], F32)) for i in range(6)]
    pb = [st.enter_context(nc.psum_tensor("pb%d" % i, [128, 1024], BF16)) for i in range(2)]
    rr = {"f": 0, "b": 0}

    def bank_f():
        i = rr["f"] % 6
        rr["f"] += 1
        return pf[i], "pf%d" % i

    def bank_b():
        i = rr["b"] % 2
        rr["b"] += 1
        return pb[i], "pb%d" % i

    def mm(out, lhsT, rhs, start, stop, reads, writes):
        T.op("pe", lambda h: h.matmul(out, lhsT=lhsT, rhs=rhs, start=start, stop=stop), reads, writes)

    def tr(out, in_, ident, reads, writes):
        T.op("pe", lambda h: h.transpose(out, in_, ident), reads, writes)

    def act(out, in_, func, reads, writes, bias=None, scale=None, accum=None):
        kw = {}
        if bias is not None:
            kw["bias"] = bias
        if scale is not None:
            kw["scale"] = scale
        if accum is not None:
            kw["accum_out"] = accum
        T.op("act", lambda h: h.activation(out=out, in_=in_, func=func, **kw), reads, writes)

    def ts(eng, out, in0, s1, op0, reads, writes, s2=None, op1=None):
        if op1 is None:
            T.op(eng, lambda h: h.tensor_scalar(out=out, in0=in0, scalar1=s1, scalar2=None, op0=op0), reads, writes)
        else:
            T.op(eng, lambda h: h.tensor_scalar(out=out, in0=in0, scalar1=s1, scalar2=s2, op0=op0, op1=op1), reads, writes)

    def tt(eng, out, in0, in1, op, reads, writes):
        T.op(eng, lambda h: h.tensor_tensor(out=out, in0=in0, in1=in1, op=op), reads, writes)

    def stt(out, in0, scalar, in1, op0, op1, reads, writes):
        T.op("dve", lambda h: h.scalar_tensor_tensor(out=out, in0=in0, scalar=scalar, in1=in1, op0=op0, op1=op1), reads, writes)

    def cp(eng, out, in_, reads, writes):
        if eng == "act":
            T.op(eng, lambda h: h.activation(out=out, in_=in_, func=AF.Copy), reads, writes)
        else:
            T.op(eng, lambda h: h.tensor_copy(out=out, in_=in_), reads, writes)

    def memset(eng, ap, val, writes):
        T.op(eng, lambda h: h.memset(ap, val), (), writes)

    def recip(out, in_, reads, writes):
        T.op("dve", lambda h: h.reciprocal(out=out, in_=in_), reads, writes)

    def reduce_add(out, in_, reads, writes):
        T.op("dve", lambda h: h.tensor_reduce(out=out, in_=in_, axis=AX.X, op=ALU.add), reads, writes)

    def dma(q, out, in_, slot, reads, writes, slow=False):
        if slow:
            T.dma(q, lambda h: h.dma_start(out=out, in_=in_, allow_slow_non_contiguous=True), slot, reads, writes)
        else:
            T.dma(q, lambda h: h.dma_start(out=out, in_=in_), slot, reads, writes)

    def asel(out, in_, pattern, cmp_op, fill, base, cm, reads, writes):
        T.op("pool", lambda h: h.affine_select(out=out, in_=in_, pattern=pattern, compare_op=cmp_op,
                                               fill=fill, base=base, channel_multiplier=cm), reads, writes)

    memset("pool", onesb[:], 1.0, ["onesb"])
    memset("pool", acc[:], 0.0, ["acc"])
    memset("dve", ones_f[:], 1.0, ["ones_f"])
    ob4 = onesb[:].rearrange("p (a b) -> p a b", b=128)
    asel(tri_f[:], onesb[:, 0:128], [[1, 128]], ALU.is_ge, 0.0, 0, -1, ["onesb"], ["tri_f"])
    asel(mask4[:], ob4, [[0, 4], [1, 128]], ALU.is_ge, 0.0, 0, -1, ["onesb"], ["mask4"])
    asel(ident_f[:], onesb[:, 0:128], [[-1, 128]], ALU.is_equal, 0.0, 0, 1, ["onesb"], ["ident_f"])
    asel(ident_bf[:], onesb[:, 0:128], [[-1, 128]], ALU.is_equal, 0.0, 0, 1, ["onesb"], ["ident_bf"])
    ob64 = onesb[:, 0:256].rearrange("p (a b) -> p a b", b=64)
    for hp in range(2):
        asel(mask64[hp * 64:(hp + 1) * 64], ob64[hp * 64:(hp + 1) * 64], [[0, 4], [1, 64]], ALU.is_ge, 0.0, 0, -1,
             ["onesb"], ["mask64"])
    zsrc = zsrc_t[:, 0:512].rearrange("p (a b) -> p a b", b=128)
    asel(nm_cur[:], zsrc, [[0, 4], [1, 128]], ALU.is_ge, NEG, 0, -1, ["acc", "ftmp"], ["nm_cur"])
    asel(nm_prev[:], zsrc, [[0, 4], [-1, 128]], ALU.is_ge, NEG, -1, 1, ["acc", "ftmp"], ["nm_prev"])
    memset("dve", Vm[:], 1.0, ["Vm"])
    memset("dve", Vs[:], 1.0, ["Vs"])
    memset("dve", Vscar[:], 1.0, ["Vscar"])
    memset("dve", KTcar[:], 0.0, ["KTcar"])
    memset("pool", Vh0[:], 0.0, ["Vh0"])
    memset("pool", Vh1[:], 0.0, ["Vh1"])
    memset("pool", QT0[:], 0.0, ["QT0"])
    memset("pool", QT1[:], 0.0, ["QT1"])
    memset("pool", qbd_m[:], 0.0, ["qbd_m"])
    memset("pool", qbd_h[:], 0.0, ["qbd_h"])
    memset("pool", hSbd[:], 0.0, ["hSbd"])
    memset("pool", mCbd[:], 0.0, ["mCbd"])
    memset("dve", Gc[:], 0.0, ["Gc"])

    nrow1 = 3 * DEPTH * KC
    for i, src in enumerate((in_norm_g, mlp_norm_g, ple_norm_g)):
        dma("sp", stg1[i * DEPTH * KC:(i + 1) * DEPTH * KC, 0:128], src.rearrange("l (c p) -> (l c) p", p=128),
            "stg1", [], ["t256"])
    b1, b1n = bank_f()
    tr(b1[:, 0:nrow1], stg1[0:nrow1, 0:128], ident_f[0:nrow1, 0:nrow1], ["t256", "ident_f"], [b1n])
    cp("dve", gcols[:].rearrange("p a l c -> p (a l c)"), b1[:, 0:nrow1], [b1n], ["gcols"])
    n_cw = DEPTH * 4 * 4
    n_cb = DEPTH * 4
    n_lb = DEPTH * 2
    dma("sp", stg2[0:n_cw, 0:128], conv_w.rearrange("l j (c p) -> (l j c) p", p=128), "stg2", [], ["u256"])
    dma("sp", stg2[n_cw:n_cw + n_cb, 0:128], conv_b.rearrange("l (c p) -> (l c) p", p=128), "stg2", [], ["u256"])
    dma("sp", stg2[n_cw + n_cb:n_cw + n_cb + n_lb, 0:128], lb_logits.rearrange("l (c p) -> (l c) p", p=128),
        "stg2", [], ["u256"])
    nrow2 = n_cw + n_cb + n_lb
    b2, b2n = bank_f()
    tr(b2[:, 0:nrow2], stg2[0:nrow2, 0:128], ident_f[0:nrow2, 0:nrow2], ["u256", "ident_f"], [b2n])
    cp("dve", cwcol[:].rearrange("p l j c -> p (l j c)"), b2[:, 0:n_cw], [b2n], ["cwcol"])
    cp("dve", cbcol[:].rearrange("p l c -> p (l c)"), b2[:, n_cw:n_cw + n_cb], [b2n], ["cbcol"])
    cp("dve", lbcol[:].rearrange("p l c -> p (l c)"), b2[:, n_cw + n_cb:nrow2], [b2n], ["lbcol"])
    act(lbe[:], lbcol[:], AF.Exp, ["lbcol"], ["lbe"])
    reduce_add(sm[:, 0:2], lbe[:].rearrange("p l c -> p c l"), ["lbe"], ["sm"])
    recip(sm[:, 0:2], sm[:, 0:2], ["sm"], ["sm"])
    tt("dve", lbe[:], lbe[:], sm[:, 0:2].unsqueeze(1).to_broadcast([128, DEPTH, 2]), ALU.mult, ["lbe", "sm"], ["lbe"])
    memset("dve", lbcol[:, 0, :], 0.0, ["lbcol"])
    for l in range(1, DEPTH):
        tt("dve", lbcol[:, l, :], lbcol[:, l - 1, :], lbe[:, l, :], ALU.add, ["lbcol", "lbe"], ["lbcol"])
    ts("dve", omlb[:], lbcol[:], -1.0, ALU.mult, ["lbcol"], ["omlb"], 1.0, ALU.add)
    def layer_blocks(l):
        bl = []
        bl.append(("B", [(bo, n, w_in[l][:, oo:oo + n]) for (bo, oo, n) in B_PIECES], KC))
        bl.append(("A1", [(ao, n, w_in[l][:, oo:oo + n]) for (ao, oo, n) in A_PIECES if ao < 1024], KC))
        bl.append(("A2", [(ao - 1024, n, w_in[l][:, oo:oo + n]) for (ao, oo, n) in A_PIECES if ao >= 1024], KC))
        bl.append(("O", [(0, 1024, w_out[l][:, :])], KC))
        for j in range(4):
            bl.append(("U%d" % j, [(0, 1024, w_up[l][:, j * 1024:(j + 1) * 1024])], KC))
            bl.append(("D%d" % j, [(0, 1024, w_down[l][j * 1024:(j + 1) * 1024, :])], KC))
        bl.append(("G", [(0, 1024, w_gate[l][:, :])], KC))
        bl.append(("P", [(0, 1024, w_proj[l][:, :])], 2))
        return bl

    for l in range(DEPTH):
        for bi, (bname, pieces, nch) in enumerate(layer_blocks(l)):
            img = wblk[l, bi].rearrange("p (c n) -> p c n", n=1024)
            for (co, n, src) in pieces:
                T.dma("pool", lambda h, img=img, co=co, n=n, src=src, nch=nch: h.dma_start(
                    out=img[:, 0:nch, co:co + n], in_=src.rearrange("(c p) n -> p c n", p=128)),
                    "wblk%d" % l, [], ["wblk%d" % l])

    blocks = []
    for seq in range(NSEQ):
        for sg in range(NSEGS):
            for l in range(DEPTH):
                for bi, (bname, pieces, nch) in enumerate(layer_blocks(l)):
                    ncol = max(co + n for (co, n, _) in pieces)
                    blocks.append(((seq, sg, l, bname), l, bi, nch, ncol))
    ws = {"next": 0, "free": [True] * 4, "loc": {}}

    def pump():
        while ws["next"] < len(blocks):
            s = ws["next"] % 4
            if not ws["free"][s]:
                break
            key_, wl, bi, nch, ncol = blocks[ws["next"]]
            if ncol == 1024:
                dma("sp", slots[s][:, 0:nch, :].rearrange("p c n -> p (c n)"), wblk[wl, bi][:, 0:nch * 1024],
                    "slot%d" % s, ["wblk%d" % wl], ["slot%d" % s])
            else:
                dma("sp", slots[s][:, 0:nch, 0:ncol],
                    wblk[wl, bi].rearrange("p (c n) -> p c n", n=1024)[:, 0:nch, 0:ncol],
                    "slot%d" % s, ["wblk%d" % wl], ["slot%d" % s])
            ws["free"][s] = False
            ws["loc"][key_] = s
            ws["next"] += 1

    def wslot(key_):
        s = ws["loc"][key_]
        return slots[s], "slot%d" % s

    def release(key_):
        s = ws["loc"].pop(key_)
        ws["free"][s] = True
        pump()

    pump()

    inv_freq = [ROPE_THETA ** (-(2.0 * j) / 16.0) for j in range(8)]

    def norm_tiles(tiles, gsel, l, col0):
        cp("dve", grep, gcols[:, gsel, l, :].unsqueeze(2).to_broadcast([128, KC, 128]), ["gcols"], ["sqk", "sqt"])
        for i, t in enumerate(tiles):
            xr = "xres%d" % t
            act(xn[:], xres[:, t, :], AF.Square, [xr], ["xn", "sm"], accum=sm[:, 0:1])
            act(sm[:, 1:2], sm[:, 0:1], AF.Sqrt, ["sm"], ["sm"], bias=EPS, scale=1.0 / D)
            recip(sm[:, 2:3], sm[:, 1:2], ["sm"], ["sm"])
            act(xn[:], xres[:, t, :], AF.Copy, [xr, "sm"], ["xn"], scale=sm[:, 2:3])
            bk, bkn = bank_b()
            for c in range(KC):
                tr(bk[:, c * 128:(c + 1) * 128], xn[:, c * 128:(c + 1) * 128], ident_bf[:], ["xn", "ident_bf"], [bkn])
            c0 = col0 + i * 128
            tt("dve", hT[:, :, c0:c0 + 128], bk[:, :].rearrange("p (c t) -> p c t", t=128), grep, ALU.mult,
               [bkn, "sqk", "sqt"], ["hT"])

    for seq in range(NSEQ):
        for sg in range(NSEGS):
            tok0 = seq * S + sg * SEG
            first_seg = (sg == 0)
            for t in range(T_):
                dma("sp", xres[:, t, :], x_d[tok0 + t * 128: tok0 + (t + 1) * 128, :], "xres",
                    [], ["xres%d" % t])
            dma("sp", post[:], pos_d[tok0:tok0 + SEG].rearrange("(t p) -> p t", p=128), "post", [], ["post"], slow=True)
            cp("dve", posf[:], post[:], ["post"], ["posf"])
            for j in range(8):
                ts("dve", angk[:, :, j], posf[:], float(np.float32(inv_freq[j])), ALU.mult, ["posf"], ["angk"])
            for shift, dst, dstn in ((0.0, rope_s, "rope_s"), (math.pi / 2.0, rope_c, "rope_c")):
                ts("dve", angm[:], angk[:], shift, ALU.add, ["angk"], ["angm"])
                ts("dve", angf[:], angm[:], 1.0 / TWO_PI, ALU.mult, ["angm"], ["angf"])
                cp("dve", angi[:], angf[:], ["angf"], ["angi"])
                cp("dve", angf[:], angi[:], ["angi"], ["angf"])
                stt(angf[:], angf[:], -TWO_PI, angm[:], ALU.mult, ALU.add, ["angf", "angm"], ["angf"])
                ts("dve", angw[:], angf[:], math.pi, ALU.is_gt, ["angf"], ["angw"], -TWO_PI, ALU.mult)
                tt("dve", angf[:], angf[:], angw[:], ALU.add, ["angf", "angw"], ["angf"])
                ts("dve", angw[:], angf[:], -math.pi, ALU.is_lt, ["angf"], ["angw"], TWO_PI, ALU.mult)
                tt("dve", angf[:], angf[:], angw[:], ALU.add, ["angf", "angw"], ["angf"])
                ts("dve", angf[:], angf[:], 3.1415925, ALU.min, ["angf"], ["angf"], -3.1415925, ALU.max)
                act(dst[:], angf[:], AF.Sin, ["angf"], [dstn])

            if first_seg:
                memset("dve", mC[:], 0.0, ["mC"])
                memset("dve", hS[:], 0.0, ["hS"])
                memset("dve", tails[:], 0.0, ["tails"])
                memset("pool", mCbd[:], 0.0, ["mCbd"])

            for l in range(DEPTH):
                k0 = (seq, sg, l)
                dma("sp", gm_b[:], m_norm_g[l:l + 1, :].partition_broadcast(128), "gm_b", [], ["gm_b"])
                dma("sp", gh_b[:], h_norm_g[l:l + 1, :].partition_broadcast(128), "gh_b", [], ["gh_b"])
                for j in range(8):
                    dma("sp", gqk_b[:, j, :], q_norm_g[l:l + 1, :].partition_broadcast(128), "gqk_b", [], ["gqk_b"])
                for j in range(8, 10):
                    dma("sp", gqk_b[:, j, :], k_norm_g[l:l + 1, :].partition_broadcast(128), "gqk_b", [], ["gqk_b"])
                dma("sp", fb_b[:], f_bias[l:l + 1, :].partition_broadcast(128), "fb_b", [], ["fb_b"])
                dma("sp", esink[:], sinks_d[l:l + 1, :].partition_broadcast(128), "esink", [], ["esink"])
                for (ao, oo, n) in A_PIECES:
                    dma("pool", brow_a[0:1, ao:ao + n], b_in[l:l + 1, oo:oo + n], "brow_a", [], ["brow_a"])
                for (bo, oo, n) in B_PIECES:
                    dma("pool", brow_b[0:1, bo:bo + n], b_in[l:l + 1, oo:oo + n], "brow_b", [], ["brow_b"])
                act(esink[:], esink[:], AF.Exp, ["esink"], ["esink"])

                sB, sBn = wslot(k0 + ("B",))
                sA1, sA1n = wslot(k0 + ("A1",))
                sA2, sA2n = wslot(k0 + ("A2",))
                sO, sOn = wslot(k0 + ("O",))

                for g in range(NG):
                    tiles = [g * GT + i for i in range(GT)]
                    norm_tiles(tiles, 0, l, 0)
                    cp("dve", pre[:, :, 0:3], tails[:, l, :, :], ["tails"], ["pre"])
                    for i in range(8):
                        ps, psn = bank_f()
                        for c in range(KC):
                            mm(ps[:, 0:GN], sB[:, c, i * 128:(i + 1) * 128], hT[:, c, 0:GN], c == 0, False,
                               [sBn, "hT"], [psn])
                        mm(ps[:, 0:GN], brow_b[0:1, i * 128:(i + 1) * 128], ones_row[0:1, 0:GN], False, True,
                           ["brow_b", "onesb"], [psn])
                        if i < 4:
                            act(pre[:, i, 3:3 + GN], ps[:, 0:GN], AF.Copy, [psn], ["pre"])
                        elif i < 6:
                            act(hq_s[:, i - 4, 0:GN], ps[:, 0:GN], AF.Silu, [psn], ["hq_s"])
                        else:
                            r = i - 6
                            act(ftmp[:, 0:GN], ps[:, 0:GN], AF.Sigmoid, [psn], ["ftmp"])
                            ts("dve", ftmp[:, 0:GN], ftmp[:, 0:GN], omlb[:, l, r:r + 1], ALU.mult, ["ftmp", "omlb", "lbcol"], ["ftmp"],
                               lbcol[:, l, r:r + 1], ALU.add)
                            ts("dve", key[:, r, 0:GN], ftmp[:, 0:GN], -1.0, ALU.mult, ["ftmp"], ["key"], 1.0, ALU.add)
                            act(ftmp[:, 0:GN], ftmp[:, 0:GN], AF.Ln, ["ftmp"], ["ftmp"])
                            T.op("dve", lambda h, r=r: h.tensor_tensor_scan(
                                out=Gc[:, r, 1:1 + GN], data0=onesb[:, 0:GN], data1=ftmp[:, 0:GN], initial=0.0,
                                op0=ALU.mult, op1=ALU.add), ["ftmp", "onesb"], ["Gc"])
                    for i in range(4):
                        ts("dve", acc[:, 0:GN], pre[:, i, 0:GN], cwcol[:, l, 0, i:i + 1], ALU.mult, ["pre", "cwcol", "cbcol"],
                           ["acc"], cbcol[:, l, i:i + 1], ALU.add)
                        for j in range(1, 4):
                            stt(acc[:, 0:GN], pre[:, i, j:j + GN], cwcol[:, l, j, i:i + 1], acc[:, 0:GN], ALU.mult, ALU.add,
                                ["pre", "cwcol", "acc"], ["acc"])
                        act(qkc[:, i, :], acc[:, 0:GN], AF.Silu, ["acc"], ["qkc"])
                    cp("dve", tails[:, l, :, :], pre[:, :, GN:GN + 3], ["pre"], ["tails"])

                    for lt, t in enumerate(tiles):
                        co = lt * 128
                        xr = "xres%d" % t
                        gblk = sg * T_ + t
                        for pc in range(4):
                            n = (512, 512, 512, 264)[pc]
                            sl, sln = (sA1, sA1n) if pc < 2 else (sA2, sA2n)
                            so_ = (pc % 2) * 512
                            ps, psn = bank_f()
                            for c in range(KC):
                                mm(ps[:, 0:n], hT[:, c, co:co + 128], sl[:, c, so_:so_ + n], c == 0, False,
                                   ["hT", sln], [psn])
                            mm(ps[:, 0:n], ones_row[0:1, 0:128], brow_a[0:1, pc * 512:pc * 512 + n], False, True,
                               ["onesb", "brow_a"], [psn])
                            if pc == 0:
                                cp("dve", Vm[:, :, 0:64], ps[:, 0:256].rearrange("p (h d) -> p h d", d=64), [psn], ["Vm"])
                                act(og[:], ps[:, 256:512], AF.Sigmoid, [psn], ["og"])
                            elif pc == 1:
                                cp("dve", Vh0[0:64, :], ps[0:64, 0:256], [psn], ["Vh0"])
                                cp("dve", Vh1[64:128, :], ps[64:128, 0:256], [psn], ["Vh1"])
                                act(hgs[:], ps[:, 256:512], AF.Silu, [psn], ["hgs"])
                            elif pc == 2:
                                act(sqk[:, 0:8, :], ps[:, 0:512].rearrange("p (h d) -> p h d", d=64), AF.Copy, [psn], ["sqk"])
                            else:
                                cp("dve", sqk[:, 8:10, :], ps[:, 0:128].rearrange("p (h d) -> p h d", d=64), [psn], ["sqk"])
                                cp("dve", Vs[:, t + 1, :, 0:64], ps[:, 128:256].rearrange("p (h d) -> p h d", d=64),
                                   [psn], ["Vs%d" % (t + 1)])
                                cp("dve", gt[:], ps[:, 256:264], [psn], ["gt"])

                        def gen_mlstm():
                            tt("dve", smm[:, 4:8], gt[:, 4:8], fb_b[:], ALU.add, ["gt", "fb_b"], ["smm"])
                            yield
                            act(smm[:, 4:8], smm[:, 4:8], AF.Exp, ["smm"], ["smm"], scale=-1.0)
                            yield
                            act(smm[:, 8:12], smm[:, 4:8], AF.Ln, ["smm"], ["smm"], bias=1.0)
                            yield
                            pg, pgn = pf[0], "pf%d" % (0)
                            mm(pg[:, 0:4], tri_f[:], smm[:, 8:12], True, True, ["tri_f", "smm"], [pgn])
                            yield
                            mm(pg[:, 4:8], ones_f[:], smm[:, 8:12], True, True, ["ones_f", "smm"], [pgn])
                            yield
                            act(smm[:, 12:16], pg[:, 0:4], AF.Exp, [pgn], ["smm"], scale=-1.0)
                            yield
                            tt("dve", smm[:, 16:20], gt[:, 0:4], pg[:, 0:4], ALU.add, ["gt", pgn], ["smm"])
                            yield
                            act(smm[:, 16:20], smm[:, 16:20], AF.Exp, ["smm"], ["smm"], bias=math.log(0.125))
                            yield
                            for r in range(2):
                                act(smm[0:64, 20 + r:21 + r], pg[0:64, 4 + 2 * r:5 + 2 * r], AF.Exp, [pgn], ["smm"], scale=-1.0)
                                yield
                                act(smm[64:128, 20 + r:21 + r], pg[64:128, 5 + 2 * r:6 + 2 * r], AF.Exp, [pgn], ["smm"], scale=-1.0)
                                yield
                            tt("dve", Vt[:], Vm[:], smm[:, 16:20].unsqueeze(2).to_broadcast([128, 4, 65]), ALU.mult,
                               ["Vm", "smm"], ["Vt"])
                            yield
                            bk, bkn = bank_b()
                            for r in range(2):
                                tr(bk[:, r * 128:(r + 1) * 128], qkc[:, 2 + r, co:co + 128], ident_bf[:], ["qkc", "ident_bf"], [bkn])
                            cp("act", k_tm[:].rearrange("p r d -> p (r d)"), bk[:, 0:256], [bkn], ["k_tm"])
                            yield
                            yield
                            for r in range(2):
                                cp("pool", qbd_m[0:64, r, 0:128], qkc[0:64, r, co:co + 128], ["qkc"], ["qbd_m"])
                                yield
                                cp("pool", qbd_m[64:128, r, 128:256], qkc[64:128, r, co:co + 128], ["qkc"], ["qbd_m"])
                                yield
                            ps, psn = pf[1], "pf%d" % (1)
                            for r in range(2):
                                mm(ps[:, r * 256:(r + 1) * 256], qkc[:, 2 + r, co:co + 128], qbd_m[:, r, :], True, True,
                                   ["qkc", "qbd_m"], [psn])
                                yield
                            tt("dve", Wt[:], ps[:, :], mask4[:].rearrange("p a b -> p (a b)"), ALU.mult, [psn, "mask4"], ["Wt"])
                            yield
                            po, pon = pf[0], "pf%d" % (0)
                            for r in range(2):
                                mm(po[:, r * 130:(r + 1) * 130], qkc[:, r, co:co + 128], mCbd[:, l, r, :], True, False,
                                   ["qkc", "mCbd"], [pon])
                                yield
                                for hb in range(2):
                                    h_ = 2 * r + hb
                                    mm(po[:, h_ * 65:(h_ + 1) * 65], Wt[:, h_ * 128:(h_ + 1) * 128], Vt[:, h_, :], False,
                                       hb == 1, ["Wt", "Vt"], [pon])
                                    yield
                            pu, pun = pf[1], "pf%d" % (1)
                            for r in range(2):
                                mm(pu[:, r * 130:(r + 1) * 130], k_tm[:, r, :],
                                   Vt[:, 2 * r:2 * r + 2, :].rearrange("p h d -> p (h d)"), True, True, ["k_tm", "Vt"], [pun])
                                yield
                            tt("dve", tmpC[:], mC[:, l, :, :], smm[:, 20:22].unsqueeze(2).to_broadcast([128, 2, 65]), ALU.mult,
                               ["mC", "smm"], ["tmpC"])
                            yield
                            for r in range(2):
                                for hb in range(2):
                                    prt = slice(hb * 64, (hb + 1) * 64)
                                    stt(mC[prt, l, r, :], pu[prt, r * 130 + hb * 65:r * 130 + (hb + 1) * 65],
                                        smm[prt, 20 + r:21 + r], tmpC[prt, r, :], ALU.mult, ALU.add,
                                        [pun, "smm", "tmpC"], ["mC"])
                                    yield
                                    cp("pool", mCbd[prt, l, r, hb * 65:(hb + 1) * 65], mC[prt, l, r, :], ["mC"], ["mCbd"])
                                    yield
                            tt("dve", hr[:], po[:, 0:260].rearrange("p (h d) -> p h d", d=65),
                               smm[:, 12:16].unsqueeze(2).to_broadcast([128, 4, 65]), ALU.mult, [pon, "smm"], ["hr"])
                            yield
                            act(smm[:, 24:28], hr[:, :, 64], AF.Abs, ["hr"], ["smm"])
                            yield
                            ts("dve", smm[:, 24:28], smm[:, 24:28], 1.0, ALU.max, ["smm"], ["smm"])
                            yield
                            recip(smm[:, 24:28], smm[:, 24:28], ["smm"], ["smm"])
                            yield
                            t4 = t256[:].rearrange("p (h d) -> p h d", d=64)
                            u4 = u256[:].rearrange("p (h d) -> p h d", d=64)
                            tt("dve", t4, hr[:, :, 0:64], smm[:, 24:28].unsqueeze(2).to_broadcast([128, 4, 64]), ALU.mult,
                               ["hr", "smm"], ["t256"])
                            yield
                            tt("dve", u4, t4, t4, ALU.mult, ["t256"], ["u256"])
                            yield
                            reduce_add(smm[:, 28:32], u4, ["u256"], ["smm"])
                            yield
                            act(smm[:, 28:32], smm[:, 28:32], AF.Sqrt, ["smm"], ["smm"], bias=EPS, scale=1.0 / 64)
                            yield
                            recip(smm[:, 28:32], smm[:, 28:32], ["smm"], ["smm"])
                            yield
                            tt("dve", t4, t4, smm[:, 28:32].unsqueeze(2).to_broadcast([128, 4, 64]), ALU.mult,
                               ["t256", "smm"], ["t256"])
                            yield
                            tt("dve", t256[:], t256[:], gm_b[:], ALU.mult, ["t256", "gm_b"], ["t256"])
                            yield
                            tt("dve", mix[:, 0:256], t256[:], og[:], ALU.mult, ["t256", "og"], ["mix"])
                            yield


                        def gen_hgrn():
                            c_prev = co
                            gmid = Gc[:, :, co + 32:co + 129:64]
                            gend = Gc[:, :, co + 64:co + 129:64]
                            gprv = Gc[:, :, co:co + 65:64]
                            tt("dve", hgE[:, 0, :, :], gmid, gprv, ALU.subtract, ["Gc"], ["hgE"])
                            yield
                            tt("dve", hgE[:, 1, :, :], gend, gmid, ALU.subtract, ["Gc"], ["hgE"])
                            yield
                            tt("dve", hgE[:, 2, :, :], gend, gprv, ALU.subtract, ["Gc"], ["hgE"])
                            yield
                            act(hgE[:], hgE[:], AF.Exp, ["hgE"], ["hgE"])
                            yield
                            for r in range(2):
                                ts("dve", smh[:, 2 * r:2 * r + 2], Gc[:, r, co + 32:co + 129:64], -1.0, ALU.mult, ["Gc"], ["smh"])
                                yield
                            for r in range(2):
                                for cc in range(2):
                                    cs = co + cc * 64
                                    midc = 1 + cs + 31
                                    act(E1[:], Gc[:, r, 1 + cs:1 + cs + 64], AF.Exp, ["Gc", "smh"], ["E1"],
                                        bias=smh[:, 2 * r + cc:2 * r + cc + 1])
                                    yield
                                    tt("dve", qh[:, r, cc * 64:(cc + 1) * 64], hq_s[:, r, cs:cs + 64], E1[:], ALU.mult,
                                       ["hq_s", "E1"], ["qh"])
                                    yield
                                    act(E2[:], Gc[:, r, 1 + cs:1 + cs + 64], AF.Exp, ["Gc"], ["E2"],
                                        bias=Gc[:, r, midc:midc + 1], scale=-1.0)
                                    yield
                                    tt("dve", kh[:, r, cc * 64:(cc + 1) * 64], key[:, r, cs:cs + 64], E2[:], ALU.mult,
                                       ["key", "E2"], ["kh"])
                                    yield
                                    for hb in range(2):
                                        prt = slice(hb * 64, (hb + 1) * 64)
                                        cp("pool", qbd_h[prt, r, cc, hb * 64:(hb + 1) * 64], qh[prt, r, cc * 64:(cc + 1) * 64],
                                           ["qh"], ["qbd_h"])
                                        yield
                            bk, bkn = bank_b()
                            for r in range(2):
                                tr(bk[:, r * 128:(r + 1) * 128], kh[:, r, :], ident_bf[:], ["kh", "ident_bf"], [bkn])
                            cp("act", kh_tm[:].rearrange("p r d -> p (r d)"), bk[:, 0:256], [bkn], ["kh_tm"])
                            yield
                            yield
                            ps, psn = pf[2], "pf%d" % (2)
                            for cc in range(2):
                                for r in range(2):
                                    mm(ps[cc * 64:(cc + 1) * 64, r * 128:(r + 1) * 128], kh[:, r, cc * 64:(cc + 1) * 64],
                                       qbd_h[:, r, cc, :], True, True, ["kh", "qbd_h"], [psn])
                                    yield
                            tt("dve", At[:], ps[:, 0:256], mask64[:].rearrange("p a b -> p (a b)"), ALU.mult,
                               [psn, "mask64"], ["At"])
                            yield
                            po, pon = pf[3], "pf%d" % (3)
                            Vhs = (Vh0, Vh1)
                            Vhn = ("Vh0", "Vh1")
                            for cc in range(2):
                                cpr = slice(cc * 64, (cc + 1) * 64)
                                for hb in range(2):
                                    prt = slice(hb * 64, (hb + 1) * 64)
                                    tt("dve", hSbd[prt, :, hb * 64:(hb + 1) * 64], hS[prt, l, :, :],
                                       hgE[prt, 0, :, cc:cc + 1].to_broadcast([64, 2, 64]), ALU.mult, ["hS", "hgE"], ["hSbd"])
                                    yield
                                for r in range(2):
                                    mm(po[cpr, r * 128:(r + 1) * 128], qh[:, r, cc * 64:(cc + 1) * 64], hSbd[:, r, :], True, False,
                                       ["qh", "hSbd"], [pon])
                                    yield
                                    for hb in range(2):
                                        h_ = 2 * r + hb
                                        mm(po[cpr, h_ * 64:(h_ + 1) * 64], At[:, h_ * 64:(h_ + 1) * 64],
                                           Vhs[cc][:, h_ * 64:(h_ + 1) * 64], False, hb == 1, ["At", Vhn[cc]], [pon])
                                        yield
                                pu, pun = pf[2], "pf%d" % (2)
                                for r in range(2):
                                    mm(pu[:, r * 128:(r + 1) * 128], kh_tm[:, r, :], Vhs[cc][:, r * 128:(r + 1) * 128], True, True,
                                       ["kh_tm", Vhn[cc]], [pun])
                                    yield
                                tt("dve", tmpS[:], hS[:, l, :, :], hgE[:, 2, :, cc:cc + 1].to_broadcast([128, 2, 64]), ALU.mult,
                                   ["hS", "hgE"], ["tmpS"])
                                yield
                                for r in range(2):
                                    for hb in range(2):
                                        prt = slice(hb * 64, (hb + 1) * 64)
                                        stt(hS[prt, l, r, :], pu[prt, r * 128 + hb * 64:r * 128 + (hb + 1) * 64],
                                            hgE[prt, 1, r, cc:cc + 1], tmpS[prt, r, :], ALU.mult, ALU.add,
                                            [pun, "hgE", "tmpS"], ["hS"])
                                        yield
                            cp("act", t256h[:], po[:, 0:256], [pon], ["t256h"])
                            yield
                            tt("dve", u256h[:], t256h[:], t256h[:], ALU.mult, ["t256h"], ["u256h"])
                            yield
                            reduce_add(smh[:, 8:12], u256h[:].rearrange("p (h d) -> p h d", d=64), ["u256h"], ["smh"])
                            yield
                            act(smh[:, 8:12], smh[:, 8:12], AF.Sqrt, ["smh"], ["smh"], bias=EPS, scale=1.0 / 64)
                            yield
                            recip(smh[:, 8:12], smh[:, 8:12], ["smh"], ["smh"])
                            yield
                            tt("dve", t256h[:].rearrange("p (h d) -> p h d", d=64), t256h[:].rearrange("p (h d) -> p h d", d=64),
                               smh[:, 8:12].unsqueeze(2).to_broadcast([128, 4, 64]), ALU.mult, ["t256h", "smh"], ["t256h"])
                            yield
                            tt("dve", t256h[:], t256h[:], gh_b[:], ALU.mult, ["t256h", "gh_b"], ["t256h"])
                            yield
                            tt("dve", mix[:, 256:512], t256h[:], hgs[:], ALU.mult, ["t256h", "hgs"], ["mix"])
                            yield


                        def gen_swa():
                            tt("dve", sqt[:], sqk[:], sqk[:], ALU.mult, ["sqk"], ["sqt"])
                            yield
                            reduce_add(sms[:, 16:26], sqt[:], ["sqt"], ["sms"])
                            yield
                            act(sms[:, 16:26], sms[:, 16:26], AF.Sqrt, ["sms"], ["sms"], bias=EPS, scale=1.0 / 64)
                            yield
                            recip(sms[:, 16:26], sms[:, 16:26], ["sms"], ["sms"])
                            yield
                            tt("dve", sqk[:], sqk[:], sms[:, 16:26].unsqueeze(2).to_broadcast([128, 10, 64]), ALU.mult,
                               ["sqk", "sms"], ["sqk"])
                            yield
                            tt("dve", sqk[:], sqk[:], gqk_b[:], ALU.mult, ["sqk", "gqk_b"], ["sqk"])
                            yield
                            cosb = rope_c[:, t, :].unsqueeze(1).to_broadcast([128, 10, 8])
                            sinb = rope_s[:, t, :].unsqueeze(1).to_broadcast([128, 10, 8])
                            tt("dve", rtmp[:, 0], sqk[:, :, 0:8], cosb, ALU.mult, ["sqk", "rope_c"], ["rtmp"])
                            yield
                            tt("dve", rtmp[:, 1], sqk[:, :, 8:16], sinb, ALU.mult, ["sqk", "rope_s"], ["rtmp"])
                            yield
                            tt("dve", rtmp[:, 2], sqk[:, :, 8:16], cosb, ALU.mult, ["sqk", "rope_c"], ["rtmp"])
                            yield
                            tt("dve", rtmp[:, 3], sqk[:, :, 0:8], sinb, ALU.mult, ["sqk", "rope_s"], ["rtmp"])
                            yield
                            cp("act", qkr[:], sqk[:], ["sqk"], ["qkr"])
                            yield
                            tt("dve", qkr[:, :, 0:8], rtmp[:, 0], rtmp[:, 1], ALU.subtract, ["rtmp", "qkr"], ["qkr"])
                            yield
                            tt("dve", qkr[:, :, 8:16], rtmp[:, 2], rtmp[:, 3], ALU.add, ["rtmp", "qkr"], ["qkr"])
                            yield
                            bk, bkn = bank_b()
                            qkr2 = qkr[:].rearrange("p h d -> p (h d)")
                            for j in range(5):
                                tr(bk[:, j * 128:(j + 1) * 128], qkr2[:, j * 128:(j + 1) * 128], ident_bf[:], ["qkr", "ident_bf"], [bkn])
                            cp("act", QT0[0:64, :], bk[0:64, 0:512], [bkn], ["QT0"])
                            cp("act", QT1[64:128, :], bk[64:128, 0:512], [bkn], ["QT1"])
                            cp("dve", KT[:, (t + 1) * 128:(t + 2) * 128], bk[:, 512:640], [bkn], ["KT%d" % (t + 1)])
                            yield
                            yield
                            QTs = ((QT0, "QT0"), (QT1, "QT1"))
                            kvsrc = []
                            if gblk > 0:
                                if t == 0:
                                    kvsrc.append((KTcar[:, l, :], "KTcar", Vscar[:, l, :, :], "Vscar", nm_prev, "nm_prev"))
                                else:
                                    kvsrc.append((KT[:, t * 128:(t + 1) * 128], "KT%d" % t, Vs[:, t, :, :], "Vs%d" % t,
                                                  nm_prev, "nm_prev"))
                            kvsrc.append((KT[:, (t + 1) * 128:(t + 2) * 128], "KT%d" % (t + 1), Vs[:, t + 1, :, :],
                                          "Vs%d" % (t + 1), nm_cur, "nm_cur"))
                            pi = 0
                            for hk in range(2):
                                pts = []
                                for ki_, (kap, kn, vap, vn, nmk, nmn) in enumerate(kvsrc):
                                    ps, psn = pf[4 + ki_], "pf%d" % (4 + ki_)
                                    mm(ps[:, :], kap, QTs[hk][0][:, :], True, False, [kn, QTs[hk][1]], [psn])
                                    yield
                                    mm(ps[:, :], ident_bf[:], nmk[:].rearrange("p a b -> p (a b)"), False, True,
                                       ["ident_bf", nmn], [psn])
                                    yield
                                    ptile_ = Pt[pi % 2]
                                    ptn = "Pt%d" % (pi % 2)
                                    pi += 1
                                    act(ptile_[:], ps[:, :], AF.Exp, [psn], [ptn], scale=0.125)
                                    yield
                                    pts.append((ptile_, ptn, vap, vn))
                                po, pon = pf[4], "pf%d" % (4)
                                for g_ in range(4):
                                    for mi_, (ptile_, ptn, vap, vn) in enumerate(pts):
                                        mm(po[:, g_ * 65:(g_ + 1) * 65], ptile_[:, g_ * 128:(g_ + 1) * 128], vap[:, hk, :],
                                           mi_ == 0, mi_ == len(pts) - 1, [ptn, vn], [pon])
                                        yield
                                cp("act", so[:], po[:, 0:260].rearrange("p (h d) -> p h d", d=65), [pon], ["so"])
                                yield
                                tt("dve", sms[:, 32:36], so[:, :, 64], esink[:, hk * 4:(hk + 1) * 4], ALU.add, ["so", "esink"], ["sms"])
                                yield
                                recip(sms[:, 32:36], sms[:, 32:36], ["sms"], ["sms"])
                                yield
                                tt("dve", mix[:, 512 + hk * 256:512 + (hk + 1) * 256].rearrange("p (h d) -> p h d", d=64),
                                   so[:, :, 0:64], sms[:, 32:36].unsqueeze(2).to_broadcast([128, 4, 64]), ALU.mult,
                                   ["so", "sms"], ["mix"])
                                yield
                            if t == T_ - 1:
                                cp("pool", KTcar[:, l, :], KT[:, T_ * 128:(T_ + 1) * 128], ["KT%d" % T_], ["KTcar"])
                                yield
                                cp("pool", Vscar[:, l, :, :], Vs[:, T_, :, :], ["Vs%d" % T_], ["Vscar"])
                                yield


                        gens_ = [gen_mlstm(), gen_hgrn(), gen_swa()]
                        while gens_:
                            for g__ in list(gens_):
                                try:
                                    next(g__)
                                except StopIteration:
                                    gens_.remove(g__)

                        bk, bkn = bank_b()
                        for c in range(KC):
                            tr(bk[:, c * 128:(c + 1) * 128], mix[:, c * 128:(c + 1) * 128], ident_bf[:], ["mix", "ident_bf"], [bkn])
                        cp("act", xn[:], bk[:, :], [bkn], ["xn"])
                        for nh in range(2):
                            ps, psn = bank_f()
                            for c in range(KC):
                                mm(ps[:, :], mixT[:, c, :], sO[:, c, nh * 512:(nh + 1) * 512], c == 0, c == KC - 1,
                                   ["xn", sOn], [psn])
                            tt("dve", xres[:, t, nh * 512:(nh + 1) * 512], xres[:, t, nh * 512:(nh + 1) * 512], ps[:, :],
                               ALU.add, [xr, psn], [xr])
                release(k0 + ("B",))
                release(k0 + ("A1",))
                release(k0 + ("A2",))
                release(k0 + ("O",))

                norm_tiles(list(range(T_)), 1, l, 0)
                for j in range(4):
                    sU, sUn = wslot(k0 + ("U%d" % j,))
                    sD, sDn = wslot(k0 + ("D%d" % j,))
                    for gi in range(T_ // MG):
                        gc0 = gi * MGN
                        for i in range(KC):
                            ps, psn = bank_f()
                            for c in range(KC):
                                mm(ps[:, 0:MGN], sU[:, c, i * 128:(i + 1) * 128], hT[:, c, gc0:gc0 + MGN], c == 0, c == KC - 1,
                                   [sUn, "hT"], [psn])
                            act(sqv[:, 0:MGN], ps[:, 0:MGN], AF.Square, [psn], ["acc"])
                            stt(aT[:, i, :], ps[:, 0:MGN], 0.0, sqv[:, 0:MGN], ALU.is_gt, ALU.mult, [psn, "acc"], ["pre"])
                        for lt in range(MG):
                            t = gi * MG + lt
                            xr = "xres%d" % t
                            for nh in range(2):
                                ps, psn = bank_f()
                                for i in range(KC):
                                    mm(ps[:, :], aT[:, i, lt * 128:(lt + 1) * 128], sD[:, i, nh * 512:(nh + 1) * 512],
                                       i == 0, i == KC - 1, ["pre", sDn], [psn])
                                tt("dve", xres[:, t, nh * 512:(nh + 1) * 512], xres[:, t, nh * 512:(nh + 1) * 512], ps[:, :],
                                   ALU.add, [xr, psn], [xr])
                    release(k0 + ("U%d" % j,))
                    release(k0 + ("D%d" % j,))

                norm_tiles(list(range(T_)), 2, l, 0)
                dma("sp", gpost_b, post_g[l:l + 1, :].partition_broadcast(128), "gpost_b", [], ["key"])
                sG, sGn = wslot(k0 + ("G",))
                sP, sPn = wslot(k0 + ("P",))
                for t in range(T_):
                    xr = "xres%d" % t
                    c0 = t * 128
                    pti = ptile[t % 2]
                    ptn = ("t256", "u256")[t % 2]
                    dma("sp", pti[:], p_d[l, tok0 + c0:tok0 + c0 + 128, :], ptn, [], [ptn])
                    cp("pool", pbf[:], pti[:], [ptn], ["pbf"])
                    bk, bkn = bank_b()
                    for c in range(2):
                        tr(bk[:, c * 128:(c + 1) * 128], pbf[:, c * 128:(c + 1) * 128], ident_bf[:], ["pbf", "ident_bf"], [bkn])
                    cp("act", pT[:].rearrange("p c t -> p (c t)"), bk[:, 0:256], [bkn], ["pT"])
                    pe_banks = []
                    for nh in range(2):
                        ps, psn = bank_f()
                        for c in range(KC):
                            mm(ps[:, :], hT[:, c, c0:c0 + 128], sG[:, c, nh * 512:(nh + 1) * 512], c == 0, c == KC - 1,
                               ["hT", sGn], [psn])
                        act(gate[:, nh * 512:(nh + 1) * 512], ps[:, :], AF.Sigmoid, [psn], ["hq_s"])
                    for nh in range(2):
                        ps, psn = bank_f()
                        for c in range(2):
                            mm(ps[:, :], pT[:, c, :], sP[:, c, nh * 512:(nh + 1) * 512], c == 0, c == 1, ["pT", sPn], [psn])
                        pe_banks.append((ps, psn))
                        act(xn[:, 0:512], ps[:, :], AF.Square, [psn], ["xn", "sm2"], accum=sm2[:, 40 + nh:41 + nh])
                    tt("dve", sm2[:, 42:43], sm2[:, 40:41], sm2[:, 41:42], ALU.add, ["sm2"], ["sm2"])
                    act(sm2[:, 42:43], sm2[:, 42:43], AF.Sqrt, ["sm2"], ["sm2"], bias=EPS, scale=1.0 / D)
                    recip(sm2[:, 42:43], sm2[:, 42:43], ["sm2"], ["sm2"])
                    for nh in range(2):
                        ps, psn = pe_banks[nh]
                        stt(etmp, ps[:, :], sm2[:, 42:43], gpost_b[:, nh * 512:(nh + 1) * 512], ALU.mult, ALU.mult,
                            [psn, "sm2", "key"], ["ftmp"])
                        tt("dve", etmp, etmp, gate[:, nh * 512:(nh + 1) * 512], ALU.mult, ["ftmp", "hq_s"], ["ftmp"])
                        tt("dve", xres[:, t, nh * 512:(nh + 1) * 512], xres[:, t, nh * 512:(nh + 1) * 512], etmp, ALU.add,
                           [xr, "ftmp"], [xr])
                release(k0 + ("G",))
                release(k0 + ("P",))

            for t in range(T_):
                dma("sp", out_d[tok0 + t * 128: tok0 + (t + 1) * 128, :], xres[:, t, :], "st",
                    ["xres%d" % t], ["out%d" % t])
    T.final_wait("sp", ["out%d" % t for t in range(T_)])
    T.emit(nc, st, cfg.get("EPOCH", 32000))
    st.close()
    return nc


_PARAM_NAMES = ["in_norm_g", "w_in", "b_in", "mlstm_f_bias", "mlstm_conv_w", "mlstm_conv_b", "mlstm_norm_g",
                "hgrn_lb_logits", "hgrn_norm_g", "swa_q_norm_g", "swa_k_norm_g", "swa_sinks", "w_out", "mlp_norm_g",
                "w_up", "w_down", "ple_norm_g", "w_ple_gate", "w_ple_proj", "ple_post_norm_g"]


def make_in_maps(inputs, n_cores):
    x = np.asarray(inputs["x"])
    p = np.asarray(inputs["p"])
    pos = np.asarray(inputs["positions"])
    B, S, _ = x.shape
    depth = p.shape[0]
    nseq = B // n_cores
    params = {k: np.ascontiguousarray(np.asarray(inputs[k], dtype=np.float32)) for k in _PARAM_NAMES}
    maps = []
    for c in range(n_cores):
        sl = slice(c * nseq, (c + 1) * nseq)
        m = dict(params)
        m["x"] = np.ascontiguousarray(x[sl].reshape(nseq * S, D))
        m["p"] = np.ascontiguousarray(p[:, sl].reshape(depth, nseq * S, PLE))
        m["positions"] = np.ascontiguousarray(pos[sl].reshape(nseq * S).astype(np.int32))
        maps.append(m)
    return maps, nseq


N_LAUNCH = 1


def kernel(**inputs):
    x = np.asarray(inputs["x"])
    B, S, _ = x.shape
    depth = np.asarray(inputs["p"]).shape[0]
    nseq_total = B // N_CORES
    per = nseq_total // N_LAUNCH
    out = np.zeros((B, S, D), dtype=np.float32)
    cfg = dict(S=S, SEG=min(1024, S), GT=4, DEPTH=depth, NSEQ=per)
    nc = build_program(cfg)
    xs = x.reshape(N_CORES, nseq_total, S, D)
    ps = np.asarray(inputs["p"]).reshape(depth, N_CORES, nseq_total, S, PLE)
    pos = np.asarray(inputs["positions"]).reshape(N_CORES, nseq_total, S)
    for li in range(N_LAUNCH):
        sl = slice(li * per, (li + 1) * per)
        sub = dict(inputs)
        sub["x"] = xs[:, sl].reshape(N_CORES * per, S, D)
        sub["p"] = ps[:, :, sl].reshape(depth, N_CORES * per, S, PLE)
        sub["positions"] = pos[:, sl].reshape(N_CORES * per, S)
        maps, nseq = make_in_maps(sub, N_CORES)
        res = run_bass_kernel_spmd(nc, maps, core_ids=list(range(N_CORES)))
        o = out.reshape(N_CORES, nseq_total, S, D)
        for c, r in enumerate(res.results):
            o[c, sl] = np.asarray(r["out"]).reshape(per, S, D)
    return out
```

```python
import contextlib
import math

import numpy as np
import concourse.bass as bass
import concourse.mybir as mybir
from concourse.bass_utils import run_bass_kernel_spmd

F32 = mybir.dt.float32
BF16 = mybir.dt.bfloat16
I32 = mybir.dt.int32
AF = mybir.ActivationFunctionType
ALU = mybir.AluOpType
AX = mybir.AxisListType

N_CORES = 8
D = 1024
KC = 8
DFF = 4096
PLE = 256
IN_W = 2824
EPS = 1e-6
ROPE_THETA = 500000.0
NEG = -30000.0
TWO_PI = 2.0 * math.pi

O_MQ, O_MK, O_MV, O_MO, O_MI, O_MF = 0, 256, 512, 768, 1024, 1028
O_HQ, O_HF, O_HI, O_HG = 1032, 1288, 1544, 1800
O_SQ, O_SK, O_SV = 2056, 2568, 2696
QPERM = [0, 4, 1, 5, 2, 6, 3, 7]
A_PIECES = [(0, O_MV, 256), (256, O_MO, 256), (512, O_HI, 256), (768, O_HG, 256)]
A_PIECES += [(1024 + j * 64, O_SQ + QPERM[j] * 64, 64) for j in range(8)]
A_PIECES += [(1536, O_SK, 128), (1664, O_SV, 128), (1792, O_MI, 4), (1796, O_MF, 4)]
A_W = 1800
B_PIECES = [(0, O_MQ, 256), (256, O_MK, 256), (512, O_HQ, 256), (768, O_HF, 256)]
B_W = 1024

ENGS = ("pe", "act", "dve", "pool", "sp")


class Tracker:
    def __init__(self):
        self.streams = {e: [] for e in ENGS}
        self.count = {e: 0 for e in ENGS}
        self.waited = {e: {} for e in ENGS}
        self.last_w = {}
        self.readers = {}
        self.dma_cum = {}

    def _need(self, eng, deps, pe_skip=True):
        best = {}
        for d in deps:
            if d is None:
                continue
            kind, key, n = d
            if kind == "e" and key == eng and eng == "pe" and pe_skip:
                continue
            if kind == "d":
                n = self.dma_cum[key]
            k = (kind, key)
            if n > best.get(k, 0):
                best[k] = n
        for k, n in best.items():
            if n > self.waited[eng].get(k, 0):
                self.waited[eng][k] = n
                self.streams[eng].append(("wait", k, n))

    def _deps_for(self, reads, writes):
        deps = []
        for r in reads:
            deps.append(self.last_w.get(r))
        for w in writes:
            deps.append(self.last_w.get(w))
            deps.extend(self.readers.get(w, ()))
        return deps

    def _commit(self, me, reads, writes):
        for r in reads:
            self.readers.setdefault(r, []).append(me)
        for w in writes:
            self.last_w[w] = me
            self.readers[w] = []

    def op(self, eng, fn, reads=(), writes=()):
        self._need(eng, self._deps_for(reads, writes))
        self.count[eng] += 1
        me = ("e", eng, self.count[eng])
        self.streams[eng].append(("ins", fn, None))
        self._commit(me, reads, writes)

    def dma(self, q, fn, slot, reads=(), writes=()):
        self._need(q, self._deps_for(reads, writes))
        self.dma_cum[slot] = self.dma_cum.get(slot, 0) + 16
        me = ("d", slot, self.dma_cum[slot])
        self.streams[q].append(("dma", fn, slot))
        self._commit(me, reads, writes)

    def final_wait(self, eng, resources):
        deps = []
        for r in resources:
            deps.append(self.last_w.get(r))
            deps.extend(self.readers.get(r, ()))
        self._need(eng, deps, pe_skip=False)

    def emit(self, nc, stack, epoch=32000):
        sems = {}
        for e in ENGS:
            for k in range(self.count[e] // epoch + 1):
                sems[("e", e, k)] = stack.enter_context(nc.semaphore("s_%s%d" % (e, k)))
        for i, slot in enumerate(self.dma_cum):
            sems[("d", slot)] = stack.enter_context(nc.semaphore("d%d" % i))
        block = stack.enter_context(nc.Block())
        hmap = {"pe": block.tensor, "act": block.scalar, "dve": block.vector,
                "pool": block.gpsimd, "sp": block.sync}

        def make(e):
            def body(h):
                n_ins = 0
                for kind, a, b in self.streams[e]:
                    if kind == "wait":
                        if a[0] == "e":
                            h.wait_ge(sems[("e", a[1], (b - 1) // epoch)], (b - 1) % epoch + 1)
                        else:
                            h.wait_ge(sems[a], b)
                    elif kind == "ins":
                        a(h).then_inc(sems[("e", e, n_ins // epoch)], 1)
                        n_ins += 1
                    else:
                        a(h).then_inc(sems[("d", b)], 16)
            return body

        for e in ENGS:
            if self.streams[e]:
                hmap[e](make(e))


def build_program(cfg):
    S = cfg["S"]
    SEG = cfg["SEG"]
    GT = cfg["GT"]
    DEPTH = cfg["DEPTH"]
    NSEQ = cfg["NSEQ"]
    T_ = SEG // 128
    NSEGS = S // SEG
    NG = T_ // GT
    GN = GT * 128
    MG = min(4, T_)
    MGN = MG * 128
    NTOK = NSEQ * S

    nc = bass.Bass("TRN2", target_bir_lowering=False)
    dram = {}

    def din(name, shape, dt=F32):
        dram[name] = nc.dram_tensor(name, list(shape), dt, kind="ExternalInput").ap()
        return dram[name]

    x_d = din("x", [NTOK, D])
    p_d = din("p", [DEPTH, NTOK, PLE])
    pos_d = din("positions", [NTOK], I32)
    in_norm_g = din("in_norm_g", [DEPTH, D])
    w_in = din("w_in", [DEPTH, D, IN_W])
    b_in = din("b_in", [DEPTH, IN_W])
    f_bias = din("mlstm_f_bias", [DEPTH, 4])
    conv_w = din("mlstm_conv_w", [DEPTH, 4, 512])
    conv_b = din("mlstm_conv_b", [DEPTH, 512])
    m_norm_g = din("mlstm_norm_g", [DEPTH, 256])
    lb_logits = din("hgrn_lb_logits", [DEPTH, 256])
    h_norm_g = din("hgrn_norm_g", [DEPTH, 256])
    q_norm_g = din("swa_q_norm_g", [DEPTH, 64])
    k_norm_g = din("swa_k_norm_g", [DEPTH, 64])
    sinks_d = din("swa_sinks", [DEPTH, 8])
    w_out = din("w_out", [DEPTH, D, D])
    mlp_norm_g = din("mlp_norm_g", [DEPTH, D])
    w_up = din("w_up", [DEPTH, D, DFF])
    w_down = din("w_down", [DEPTH, DFF, D])
    ple_norm_g = din("ple_norm_g", [DEPTH, D])
    w_gate = din("w_ple_gate", [DEPTH, D, D])
    w_proj = din("w_ple_proj", [DEPTH, PLE, D])
    post_g = din("ple_post_norm_g", [DEPTH, D])
    out_d = nc.dram_tensor("out", [NTOK, D], F32, kind="ExternalOutput").ap()

    T = Tracker()
    st = contextlib.ExitStack()

    NBLK = 14
    wblk = nc.dram_tensor("wblk", [DEPTH, NBLK, 128, KC * 1024], BF16, kind="Internal").ap()

    def sb(name, shape, dt=F32):
        return st.enter_context(nc.sbuf_tensor(name, list(shape), dt))

    xres = sb("xres", [128, T_, D])
    slots = [sb("slot%d" % i, [128, KC, 1024], BF16) for i in range(4)]
    hT = sb("hT", [128, KC, SEG], BF16)
    xn = sb("xn", [128, D], BF16)
    PREW = max(GN, MGN) + 3
    pre = sb("pre", [128, 4, PREW])
    GNA = max(GN, 512)
    acc = sb("acc", [128, GNA])
    qkc = sb("qkc", [128, 4, GN], BF16)
    hq_s = sb("hq_s", [128, 2, GNA])
    Gc = sb("Gc", [128, 2, GN + 1])
    key = sb("key", [128, 2, GNA])
    ftmp = sb("ftmp", [128, GNA])
    tails = sb("tails", [128, DEPTH, 4, 3])
    og = sb("og", [128, 256])
    hgs = sb("hgs", [128, 256])
    Vm = sb("Vm", [128, 4, 65], BF16)
    Vt = sb("Vt", [128, 4, 65], BF16)
    Vh0 = sb("Vh0", [128, 256], BF16)
    Vh1 = sb("Vh1", [128, 256], BF16)
    sq2 = sb("sq2", [128, 2, 10, 64])
    sqk = sq2[:, 0]
    sqt = sq2[:, 1]
    gt = sb("gt", [128, 8])
    grep = sq2[:].rearrange("p a h d -> p (a h d)")[:, 0:KC * 128].rearrange("p (c t) -> p c t", t=128)
    Vs = sb("Vs", [128, T_ + 1, 2, 65], BF16)
    KT = sb("KT", [128, (T_ + 1) * 128], BF16)
    KTcar = sb("KTcar", [128, DEPTH, 128], BF16)
    Vscar = sb("Vscar", [128, DEPTH, 2, 65], BF16)
    QT0 = sb("QT0", [128, 512], BF16)
    QT1 = sb("QT1", [128, 512], BF16)
    Pt = [sb("Pt%d" % i, [128, 512], BF16) for i in range(2)]
    Wt = sb("Wt", [128, 512], BF16)
    At = sb("At", [128, 256], BF16)
    k_tm = sb("k_tm", [128, 2, 128], BF16)
    kh_tm = sb("kh_tm", [128, 2, 128], BF16)
    qbd_m = sb("qbd_m", [128, 2, 256], BF16)
    qh = sb("qh", [128, 2, 128], BF16)
    kh = sb("kh", [128, 2, 128], BF16)
    qbd_h = sb("qbd_h", [128, 2, 2, 128], BF16)
    E1 = sb("E1", [128, 64])
    E2 = sb("E2", [128, 64])
    mix = sb("mix", [128, D], BF16)
    xnB = mix
    mixT = xn[:].rearrange("p (c t) -> p c t", t=128)
    mC = sb("mC", [128, DEPTH, 2, 65])
    mCbd = sb("mCbd", [128, DEPTH, 2, 130], BF16)
    tmpC = sb("tmpC", [128, 2, 65])
    hS = sb("hS", [128, DEPTH, 2, 64])
    hSbd = sb("hSbd", [128, 2, 128], BF16)
    tmpS = sb("tmpS", [128, 2, 64])
    hr = sb("hr", [128, 4, 65])
    t256 = sb("t256", [128, 256])
    u256 = sb("u256", [128, 256])
    so = sb("so", [128, 4, 65])
    smm = sb("smm", [128, 64])
    smh = sb("smh", [128, 64])
    sms = sb("sms", [128, 64])
    t256h = sb("t256h", [128, 256])
    u256h = sb("u256h", [128, 256])
    sm = sb("sm", [128, 64])
    sm2 = sb("sm2", [128, 64])
    hgE = sb("hgE", [128, 3, 2, 2])
    rope_c = sb("rope_c", [128, T_, 8])
    rope_s = sb("rope_s", [128, T_, 8])
    rtmp = sb("rtmp", [128, 4, 10, 8])
    qkr = sb("qkr", [128, 10, 64], BF16)
    ident_bf = sb("ident_bf", [128, 128], BF16)
    ident_f = sb("ident_f", [128, 128])
    tri_f = sb("tri_f", [128, 128])
    ones_f = sb("ones_f", [128, 128])
    onesb = sb("onesb", [128, 512], BF16)
    mask4 = sb("mask4", [128, 4, 128], BF16)
    mask64 = sb("mask64", [128, 4, 64], BF16)
    nm_cur = sb("nm_cur", [128, 4, 128], BF16)
    nm_prev = sb("nm_prev", [128, 4, 128], BF16)
    ones_row = onesb
    brow_a = sb("brow_a", [1, A_W], BF16)
    brow_b = sb("brow_b", [1, B_W], BF16)
    stg1 = t256
    stg2 = u256
    gcols = sb("gcols", [128, 3, DEPTH, KC])
    cwcol = sb("cwcol", [128, DEPTH, 4, 4])
    cbcol = sb("cbcol", [128, DEPTH, 4])
    lbcol = sb("lbcol", [128, DEPTH, 2])
    lbe = sb("lbe", [128, DEPTH, 2])
    omlb = sb("omlb", [128, DEPTH, 2])
    gm_b = sb("gm_b", [128, 256])
    gh_b = sb("gh_b", [128, 256])
    gqk_b = sb("gqk_b", [128, 10, 64])
    fb_b = sb("fb_b", [128, 4])
    esink = sb("esink", [128, 8])
    aT = pre[:].rearrange("p a b -> p (a b)").bitcast(BF16)[:, 0:KC * MGN].rearrange("p (c n) -> p c n", n=MGN)
    sqv = acc
    ptile = [t256, u256]
    gpost_b = key[:, :, 0:512].rearrange("p r n -> p (r n)")
    gate = hq_s[:, :, 0:512].rearrange("p r n -> p (r n)")
    etmp = ftmp[:, 0:512]
    zsrc_t = acc
    pbf = sb("pbf", [128, PLE], BF16)
    pT = sb("pT", [128, 2, 128], BF16)
    post = sb("post", [128, T_], I32)
    posf = sb("posf", [128, T_])
    angk = sb("angk", [128, T_, 8])
    angi = sb("angi", [128, T_, 8], I32)
    angf = sb("angf", [128, T_, 8])
    angm = sb("angm", [128, T_, 8])
    angw = sb("angw", [128, T_, 8])

    pf = [st.enter_context(nc.psum_tensor("pf%d" % i, [128, 512], F32)) for i in range(6)]
    pb = [st.enter_context(nc.psum_tensor("pb%d" % i, [128, 1024], BF16)) for i in range(2)]
    rr = {"f": 0, "b": 0}

    def bank_f():
        i = rr["f"] % 6
        rr["f"] += 1
        return pf[i], "pf%d" % i

    def bank_b():
        i = rr["b"] % 2
        rr["b"] += 1
        return pb[i], "pb%d" % i

    def mm(out, lhsT, rhs, start, stop, reads, writes):
        T.op("pe", lambda h: h.matmul(out, lhsT=lhsT, rhs=rhs, start=start, stop=stop), reads, writes)

    def tr(out, in_, ident, reads, writes):
        T.op("pe", lambda h: h.transpose(out, in_, ident), reads, writes)

    def act(out, in_, func, reads, writes, bias=None, scale=None, accum=None):
        kw = {}
        if bias is not None:
            kw["bias"] = bias
        if scale is not None:
            kw["scale"] = scale
        if accum is not None:
            kw["accum_out"] = accum
        T.op("act", lambda h: h.activation(out=out, in_=in_, func=func, **kw), reads, writes)

    def ts(eng, out, in0, s1, op0, reads, writes, s2=None, op1=None):
        if op1 is None:
            T.op(eng, lambda h: h.tensor_scalar(out=out, in0=in0, scalar1=s1, scalar2=None, op0=op0), reads, writes)
        else:
            T.op(eng, lambda h: h.tensor_scalar(out=out, in0=in0, scalar1=s1, scalar2=s2, op0=op0, op1=op1), reads, writes)

    def tt(eng, out, in0, in1, op, reads, writes):
        T.op(eng, lambda h: h.tensor_tensor(out=out, in0=in0, in1=in1, op=op), reads, writes)

    def stt(out, in0, scalar, in1, op0, op1, reads, writes):
        T.op("dve", lambda h: h.scalar_tensor_tensor(out=out, in0=in0, scalar=scalar, in1=in1, op0=op0, op1=op1), reads, writes)

    def cp(eng, out, in_, reads, writes):
        if eng == "act":
            T.op(eng, lambda h: h.activation(out=out, in_=in_, func=AF.Copy), reads, writes)
        else:
            T.op(eng, lambda h: h.tensor_copy(out=out, in_=in_), reads, writes)

    def memset(eng, ap, val, writes):
        T.op(eng, lambda h: h.memset(ap, val), (), writes)

    def recip(out, in_, reads, writes):
        T.op("dve", lambda h: h.reciprocal(out=out, in_=in_), reads, writes)

    def reduce_add(out, in_, reads, writes):
        T.op("dve", lambda h: h.tensor_reduce(out=out, in_=in_, axis=AX.X, op=ALU.add), reads, writes)

    def dma(q, out, in_, slot, reads, writes, slow=False):
        if slow:
            T.dma(q, lambda h: h.dma_start(out=out, in_=in_, allow_slow_non_contiguous=True), slot, reads, writes)
        else:
            T.dma(q, lambda h: h.dma_start(out=out, in_=in_), slot, reads, writes)

    def asel(out, in_, pattern, cmp_op, fill, base, cm, reads, writes):
        T.op("pool", lambda h: h.affine_select(out=out, in_=in_, pattern=pattern, compare_op=cmp_op,
                                               fill=fill, base=base, channel_multiplier=cm), reads, writes)

    memset("pool", onesb[:], 1.0, ["onesb"])
    memset("pool", acc[:], 0.0, ["acc"])
    memset("dve", ones_f[:], 1.0, ["ones_f"])
    ob4 = onesb[:].rearrange("p (a b) -> p a b", b=128)
    asel(tri_f[:], onesb[:, 0:128], [[1, 128]], ALU.is_ge, 0.0, 0, -1, ["onesb"], ["tri_f"])
    asel(mask4[:], ob4, [[0, 4], [1, 128]], ALU.is_ge, 0.0, 0, -1, ["onesb"], ["mask4"])
    asel(ident_f[:], onesb[:, 0:128], [[-1, 128]], ALU.is_equal, 0.0, 0, 1, ["onesb"], ["ident_f"])
    asel(ident_bf[:], onesb[:, 0:128], [[-1, 128]], ALU.is_equal, 0.0, 0, 1, ["onesb"], ["ident_bf"])
    ob64 = onesb[:, 0:256].rearrange("p (a b) -> p a b", b=64)
    for hp in range(2):
        asel(mask64[hp * 64:(hp + 1) * 64], ob64[hp * 64:(hp + 1) * 64], [[0, 4], [1, 64]], ALU.is_ge, 0.0, 0, -1,
             ["onesb"], ["mask64"])
    zsrc = zsrc_t[:, 0:512].rearrange("p (a b) -> p a b", b=128)
    asel(nm_cur[:], zsrc, [[0, 4], [1, 128]], ALU.is_ge, NEG, 0, -1, ["acc", "ftmp"], ["nm_cur"])
    asel(nm_prev[:], zsrc, [[0, 4], [-1, 128]], ALU.is_ge, NEG, -1, 1, ["acc", "ftmp"], ["nm_prev"])
    memset("dve", Vm[:], 1.0, ["Vm"])
    memset("dve", Vs[:], 1.0, ["Vs"])
    memset("dve", Vscar[:], 1.0, ["Vscar"])
    memset("dve", KTcar[:], 0.0, ["KTcar"])
    memset("pool", Vh0[:], 0.0, ["Vh0"])
    memset("pool", Vh1[:], 0.0, ["Vh1"])
    memset("pool", QT0[:], 0.0, ["QT0"])
    memset("pool", QT1[:], 0.0, ["QT1"])
    memset("pool", qbd_m[:], 0.0, ["qbd_m"])
    memset("pool", qbd_h[:], 0.0, ["qbd_h"])
    memset("pool", hSbd[:], 0.0, ["hSbd"])
    memset("pool", mCbd[:], 0.0, ["mCbd"])
    memset("dve", Gc[:], 0.0, ["Gc"])

    nrow1 = 3 * DEPTH * KC
    for i, src in enumerate((in_norm_g, mlp_norm_g, ple_norm_g)):
        dma("sp", stg1[i * DEPTH * KC:(i + 1) * DEPTH * KC, 0:128], src.rearrange("l (c p) -> (l c) p", p=128),
            "stg1", [], ["t256"])
    b1, b1n = bank_f()
    tr(b1[:, 0:nrow1], stg1[0:nrow1, 0:128], ident_f[0:nrow1, 0:nrow1], ["t256", "ident_f"], [b1n])
    cp("dve", gcols[:].rearrange("p a l c -> p (a l c)"), b1[:, 0:nrow1], [b1n], ["gcols"])
    n_cw = DEPTH * 4 * 4
    n_cb = DEPTH * 4
    n_lb = DEPTH * 2
    dma("sp", stg2[0:n_cw, 0:128], conv_w.rearrange("l j (c p) -> (l j c) p", p=128), "stg2", [], ["u256"])
    dma("sp", stg2[n_cw:n_cw + n_cb, 0:128], conv_b.rearrange("l (c p) -> (l c) p", p=128), "stg2", [], ["u256"])
    dma("sp", stg2[n_cw + n_cb:n_cw + n_cb + n_lb, 0:128], lb_logits.rearrange("l (c p) -> (l c) p", p=128),
        "stg2", [], ["u256"])
    nrow2 = n_cw + n_cb + n_lb
    b2, b2n = bank_f()
    tr(b2[:, 0:nrow2], stg2[0:nrow2, 0:128], ident_f[0:nrow2, 0:nrow2], ["u256", "ident_f"], [b2n])
    cp("dve", cwcol[:].rearrange("p l j c -> p (l j c)"), b2[:, 0:n_cw], [b2n], ["cwcol"])
    cp("dve", cbcol[:].rearrange("p l c -> p (l c)"), b2[:, n_cw:n_cw + n_cb], [b2n], ["cbcol"])
    cp("dve", lbcol[:].rearrange("p l c -> p (l c)"), b2[:, n_cw + n_cb:nrow2], [b2n], ["lbcol"])
    act(lbe[:], lbcol[:], AF.Exp, ["lbcol"], ["lbe"])
    reduce_add(sm[:, 0:2], lbe[:].rearrange("p l c -> p c l"), ["lbe"], ["sm"])
    recip(sm[:, 0:2], sm[:, 0:2], ["sm"], ["sm"])
    tt("dve", lbe[:], lbe[:], sm[:, 0:2].unsqueeze(1).to_broadcast([128, DEPTH, 2]), ALU.mult, ["lbe", "sm"], ["lbe"])
    memset("dve", lbcol[:, 0, :], 0.0, ["lbcol"])
    for l in range(1, DEPTH):
        tt("dve", lbcol[:, l, :], lbcol[:, l - 1, :], lbe[:, l, :], ALU.add, ["lbcol", "lbe"], ["lbcol"])
    ts("dve", omlb[:], lbcol[:], -1.0, ALU.mult, ["lbcol"], ["omlb"], 1.0, ALU.add)
    def layer_blocks(l):
        bl = []
        bl.append(("B", [(bo, n, w_in[l][:, oo:oo + n]) for (bo, oo, n) in B_PIECES], KC))
        bl.append(("A1", [(ao, n, w_in[l][:, oo:oo + n]) for (ao, oo, n) in A_PIECES if ao < 1024], KC))
        bl.append(("A2", [(ao - 1024, n, w_in[l][:, oo:oo + n]) for (ao, oo, n) in A_PIECES if ao >= 1024], KC))
        bl.append(("O", [(0, 1024, w_out[l][:, :])], KC))
        for j in range(4):
            bl.append(("U%d" % j, [(0, 1024, w_up[l][:, j * 1024:(j + 1) * 1024])], KC))
            bl.append(("D%d" % j, [(0, 1024, w_down[l][j * 1024:(j + 1) * 1024, :])], KC))
        bl.append(("G", [(0, 1024, w_gate[l][:, :])], KC))
        bl.append(("P", [(0, 1024, w_proj[l][:, :])], 2))
        return bl

    for l in range(DEPTH):
        for bi, (bname, pieces, nch) in enumerate(layer_blocks(l)):
            img = wblk[l, bi].rearrange("p (c n) -> p c n", n=1024)
            for (co, n, src) in pieces:
                T.dma("pool", lambda h, img=img, co=co, n=n, src=src, nch=nch: h.dma_start(
                    out=img[:, 0:nch, co:co + n], in_=src.rearrange("(c p) n -> p c n", p=128)),
                    "wblk%d" % l, [], ["wblk%d" % l])

    blocks = []
    for seq in range(NSEQ):
        for sg in range(NSEGS):
            for l in range(DEPTH):
                for bi, (bname, pieces, nch) in enumerate(layer_blocks(l)):
                    ncol = max(co + n for (co, n, _) in pieces)
                    blocks.append(((seq, sg, l, bname), l, bi, nch, ncol))
    ws = {"next": 0, "free": [True] * 4, "loc": {}}

    def pump():
        while ws["next"] < len(blocks):
            s = ws["next"] % 4
            if not ws["free"][s]:
                break
            key_, wl, bi, nch, ncol = blocks[ws["next"]]
            if ncol == 1024:
                dma("sp", slots[s][:, 0:nch, :].rearrange("p c n -> p (c n)"), wblk[wl, bi][:, 0:nch * 1024],
                    "slot%d" % s, ["wblk%d" % wl], ["slot%d" % s])
            else:
                dma("sp", slots[s][:, 0:nch, 0:ncol],
                    wblk[wl, bi].rearrange("p (c n) -> p c n", n=1024)[:, 0:nch, 0:ncol],
                    "slot%d" % s, ["wblk%d" % wl], ["slot%d" % s])
            ws["free"][s] = False
            ws["loc"][key_] = s
            ws["next"] += 1

    def wslot(key_):
        s = ws["loc"][key_]
        return slots[s], "slot%d" % s

    def release(key_):
        s = ws["loc"].pop(key_)
        ws["free"][s] = True
        pump()

    pump()

    inv_freq = [ROPE_THETA ** (-(2.0 * j) / 16.0) for j in range(8)]

    def norm_tiles(tiles, gsel, l, col0):
        cp("dve", grep, gcols[:, gsel, l, :].unsqueeze(2).to_broadcast([128, KC, 128]), ["gcols"], ["sqk", "sqt"])
        for i, t in enumerate(tiles):
            xr = "xres%d" % t
            xb_, xbn = (xn, "xn") if i % 2 == 0 else (xnB, "mix")
            smn = "smn%d" % (i % 2)
            q0 = 48 + 4 * (i % 2)
            act(xb_[:], xres[:, t, :], AF.Square, [xr], [xbn, smn], accum=sm[:, q0:q0 + 1])
            act(sm[:, q0 + 1:q0 + 2], sm[:, q0:q0 + 1], AF.Sqrt, [smn], [smn], bias=EPS, scale=1.0 / D)
            recip(sm[:, q0 + 2:q0 + 3], sm[:, q0 + 1:q0 + 2], [smn], [smn])
            act(xb_[:], xres[:, t, :], AF.Copy, [xr, smn], [xbn], scale=sm[:, q0 + 2:q0 + 3])
            bk, bkn = bank_b()
            for c in range(KC):
                tr(bk[:, c * 128:(c + 1) * 128], xb_[:, c * 128:(c + 1) * 128], ident_bf[:], [xbn, "ident_bf"], [bkn])
            c0 = col0 + i * 128
            tt("dve", hT[:, :, c0:c0 + 128], bk[:, :].rearrange("p (c t) -> p c t", t=128), grep, ALU.mult,
               [bkn, "sqk", "sqt"], ["hT"])

    for seq in range(NSEQ):
        for sg in range(NSEGS):
            tok0 = seq * S + sg * SEG
            first_seg = (sg == 0)
            for t in range(T_):
                dma("sp", xres[:, t, :], x_d[tok0 + t * 128: tok0 + (t + 1) * 128, :], "xres",
                    [], ["xres%d" % t])
            dma("sp", post[:], pos_d[tok0:tok0 + SEG].rearrange("(t p) -> p t", p=128), "post", [], ["post"], slow=True)
            cp("dve", posf[:], post[:], ["post"], ["posf"])
            for j in range(8):
                ts("dve", angk[:, :, j], posf[:], float(np.float32(inv_freq[j])), ALU.mult, ["posf"], ["angk"])
            for shift, dst, dstn in ((0.0, rope_s, "rope_s"), (math.pi / 2.0, rope_c, "rope_c")):
                ts("dve", angm[:], angk[:], shift, ALU.add, ["angk"], ["angm"])
                ts("dve", angf[:], angm[:], 1.0 / TWO_PI, ALU.mult, ["angm"], ["angf"])
                cp("dve", angi[:], angf[:], ["angf"], ["angi"])
                cp("dve", angf[:], angi[:], ["angi"], ["angf"])
                stt(angf[:], angf[:], -TWO_PI, angm[:], ALU.mult, ALU.add, ["angf", "angm"], ["angf"])
                ts("dve", angw[:], angf[:], math.pi, ALU.is_gt, ["angf"], ["angw"], -TWO_PI, ALU.mult)
                tt("dve", angf[:], angf[:], angw[:], ALU.add, ["angf", "angw"], ["angf"])
                ts("dve", angw[:], angf[:], -math.pi, ALU.is_lt, ["angf"], ["angw"], TWO_PI, ALU.mult)
                tt("dve", angf[:], angf[:], angw[:], ALU.add, ["angf", "angw"], ["angf"])
                ts("dve", angf[:], angf[:], 3.1415925, ALU.min, ["angf"], ["angf"], -3.1415925, ALU.max)
                act(dst[:], angf[:], AF.Sin, ["angf"], [dstn])

            if first_seg:
                memset("dve", mC[:], 0.0, ["mC"])
                memset("dve", hS[:], 0.0, ["hS"])
                memset("dve", tails[:], 0.0, ["tails"])
                memset("pool", mCbd[:], 0.0, ["mCbd"])

            for l in range(DEPTH):
                k0 = (seq, sg, l)
                dma("sp", gm_b[:], m_norm_g[l:l + 1, :].partition_broadcast(128), "gm_b", [], ["gm_b"])
                dma("sp", gh_b[:], h_norm_g[l:l + 1, :].partition_broadcast(128), "gh_b", [], ["gh_b"])
                for j in range(8):
                    dma("sp", gqk_b[:, j, :], q_norm_g[l:l + 1, :].partition_broadcast(128), "gqk_b", [], ["gqk_b"])
                for j in range(8, 10):
                    dma("sp", gqk_b[:, j, :], k_norm_g[l:l + 1, :].partition_broadcast(128), "gqk_b", [], ["gqk_b"])
                dma("sp", fb_b[:], f_bias[l:l + 1, :].partition_broadcast(128), "fb_b", [], ["fb_b"])
                dma("sp", esink[:], sinks_d[l:l + 1, :].partition_broadcast(128), "esink", [], ["esink"])
                for (ao, oo, n) in A_PIECES:
                    dma("pool", brow_a[0:1, ao:ao + n], b_in[l:l + 1, oo:oo + n], "brow_a", [], ["brow_a"])
                for (bo, oo, n) in B_PIECES:
                    dma("pool", brow_b[0:1, bo:bo + n], b_in[l:l + 1, oo:oo + n], "brow_b", [], ["brow_b"])
                act(esink[:], esink[:], AF.Exp, ["esink"], ["esink"])

                sB, sBn = wslot(k0 + ("B",))
                sA1, sA1n = wslot(k0 + ("A1",))
                sA2, sA2n = wslot(k0 + ("A2",))
                sO, sOn = wslot(k0 + ("O",))

                for g in range(NG):
                    tiles = [g * GT + i for i in range(GT)]
                    norm_tiles(tiles, 0, l, 0)
                    cp("dve", pre[:, :, 0:3], tails[:, l, :, :], ["tails"], ["pre"])
                    for i in range(8):
                        ps, psn = bank_f()
                        for c in range(KC):
                            mm(ps[:, 0:GN], sB[:, c, i * 128:(i + 1) * 128], hT[:, c, 0:GN], c == 0, False,
                               [sBn, "hT"], [psn])
                        mm(ps[:, 0:GN], brow_b[0:1, i * 128:(i + 1) * 128], ones_row[0:1, 0:GN], False, True,
                           ["brow_b", "onesb"], [psn])
                        if i < 4:
                            act(pre[:, i, 3:3 + GN], ps[:, 0:GN], AF.Copy, [psn], ["pre"])
                        elif i < 6:
                            act(hq_s[:, i - 4, 0:GN], ps[:, 0:GN], AF.Silu, [psn], ["hq_s"])
                        else:
                            r = i - 6
                            act(ftmp[:, 0:GN], ps[:, 0:GN], AF.Sigmoid, [psn], ["ftmp"])
                            ts("dve", ftmp[:, 0:GN], ftmp[:, 0:GN], omlb[:, l, r:r + 1], ALU.mult, ["ftmp", "omlb", "lbcol"], ["ftmp"],
                               lbcol[:, l, r:r + 1], ALU.add)
                            ts("dve", key[:, r, 0:GN], ftmp[:, 0:GN], -1.0, ALU.mult, ["ftmp"], ["key"], 1.0, ALU.add)
                            act(ftmp[:, 0:GN], ftmp[:, 0:GN], AF.Ln, ["ftmp"], ["ftmp"])
                            T.op("dve", lambda h, r=r: h.tensor_tensor_scan(
                                out=Gc[:, r, 1:1 + GN], data0=onesb[:, 0:GN], data1=ftmp[:, 0:GN], initial=0.0,
                                op0=ALU.mult, op1=ALU.add), ["ftmp", "onesb"], ["Gc"])
                    for i in range(4):
                        ts("dve", acc[:, 0:GN], pre[:, i, 0:GN], cwcol[:, l, 0, i:i + 1], ALU.mult, ["pre", "cwcol", "cbcol"],
                           ["acc"], cbcol[:, l, i:i + 1], ALU.add)
                        for j in range(1, 4):
                            stt(acc[:, 0:GN], pre[:, i, j:j + GN], cwcol[:, l, j, i:i + 1], acc[:, 0:GN], ALU.mult, ALU.add,
                                ["pre", "cwcol", "acc"], ["acc"])
                        act(qkc[:, i, :], acc[:, 0:GN], AF.Silu, ["acc"], ["qkc"])
                    cp("dve", tails[:, l, :, :], pre[:, :, GN:GN + 3], ["pre"], ["tails"])

                    for lt, t in enumerate(tiles):
                        co = lt * 128
                        xr = "xres%d" % t
                        gblk = sg * T_ + t
                        for pc in range(4):
                            n = (512, 512, 512, 264)[pc]
                            sl, sln = (sA1, sA1n) if pc < 2 else (sA2, sA2n)
                            so_ = (pc % 2) * 512
                            ps, psn = bank_f()
                            for c in range(KC):
                                mm(ps[:, 0:n], hT[:, c, co:co + 128], sl[:, c, so_:so_ + n], c == 0, False,
                                   ["hT", sln], [psn])
                            mm(ps[:, 0:n], ones_row[0:1, 0:128], brow_a[0:1, pc * 512:pc * 512 + n], False, True,
                               ["onesb", "brow_a"], [psn])
                            if pc == 0:
                                cp("dve", Vm[:, :, 0:64], ps[:, 0:256].rearrange("p (h d) -> p h d", d=64), [psn], ["Vm"])
                                act(og[:], ps[:, 256:512], AF.Sigmoid, [psn], ["og"])
                            elif pc == 1:
                                cp("dve", Vh0[0:64, :], ps[0:64, 0:256], [psn], ["Vh0"])
                                cp("dve", Vh1[64:128, :], ps[64:128, 0:256], [psn], ["Vh1"])
                                act(hgs[:], ps[:, 256:512], AF.Silu, [psn], ["hgs"])
                            elif pc == 2:
                                act(sqk[:, 0:8, :], ps[:, 0:512].rearrange("p (h d) -> p h d", d=64), AF.Copy, [psn], ["sqk"])
                            else:
                                cp("dve", sqk[:, 8:10, :], ps[:, 0:128].rearrange("p (h d) -> p h d", d=64), [psn], ["sqk"])
                                cp("dve", Vs[:, t + 1, :, 0:64], ps[:, 128:256].rearrange("p (h d) -> p h d", d=64),
                                   [psn], ["Vs%d" % (t + 1)])
                                cp("dve", gt[:], ps[:, 256:264], [psn], ["gt"])

                        def gen_mlstm():
                            tt("dve", smm[:, 4:8], gt[:, 4:8], fb_b[:], ALU.add, ["gt", "fb_b"], ["smm"])
                            yield
                            act(smm[:, 4:8], smm[:, 4:8], AF.Exp, ["smm"], ["smm"], scale=-1.0)
                            yield
                            act(smm[:, 8:12], smm[:, 4:8], AF.Ln, ["smm"], ["smm"], bias=1.0)
                            yield
                            pg, pgn = pf[0], "pf%d" % (0)
                            mm(pg[:, 0:4], tri_f[:], smm[:, 8:12], True, True, ["tri_f", "smm"], [pgn])
                            yield
                            mm(pg[:, 4:8], ones_f[:], smm[:, 8:12], True, True, ["ones_f", "smm"], [pgn])
                            yield
                            act(smm[:, 12:16], pg[:, 0:4], AF.Exp, [pgn], ["smm"], scale=-1.0)
                            yield
                            tt("dve", smm[:, 16:20], gt[:, 0:4], pg[:, 0:4], ALU.add, ["gt", pgn], ["smm"])
                            yield
                            act(smm[:, 16:20], smm[:, 16:20], AF.Exp, ["smm"], ["smm"], bias=math.log(0.125))
                            yield
                            for r in range(2):
                                act(smm[0:64, 20 + r:21 + r], pg[0:64, 4 + 2 * r:5 + 2 * r], AF.Exp, [pgn], ["smm"], scale=-1.0)
                                yield
                                act(smm[64:128, 20 + r:21 + r], pg[64:128, 5 + 2 * r:6 + 2 * r], AF.Exp, [pgn], ["smm"], scale=-1.0)
                                yield
                            tt("dve", Vt[:], Vm[:], smm[:, 16:20].unsqueeze(2).to_broadcast([128, 4, 65]), ALU.mult,
                               ["Vm", "smm"], ["Vt"])
                            yield
                            bk, bkn = bank_b()
                            for r in range(2):
                                tr(bk[:, r * 128:(r + 1) * 128], qkc[:, 2 + r, co:co + 128], ident_bf[:], ["qkc", "ident_bf"], [bkn])
                            cp("act", k_tm[:].rearrange("p r d -> p (r d)"), bk[:, 0:256], [bkn], ["k_tm"])
                            yield
                            yield
                            for r in range(2):
                                cp("pool", qbd_m[0:64, r, 0:128], qkc[0:64, r, co:co + 128], ["qkc"], ["qbd_m"])
                                yield
                                cp("pool", qbd_m[64:128, r, 128:256], qkc[64:128, r, co:co + 128], ["qkc"], ["qbd_m"])
                                yield
                            ps, psn = pf[1], "pf%d" % (1)
                            for r in range(2):
                                mm(ps[:, r * 256:(r + 1) * 256], qkc[:, 2 + r, co:co + 128], qbd_m[:, r, :], True, True,
                                   ["qkc", "qbd_m"], [psn])
                                yield
                            tt("dve", Wt[:], ps[:, :], mask4[:].rearrange("p a b -> p (a b)"), ALU.mult, [psn, "mask4"], ["Wt"])
                            yield
                            po, pon = pf[0], "pf%d" % (0)
                            for r in range(2):
                                mm(po[:, r * 130:(r + 1) * 130], qkc[:, r, co:co + 128], mCbd[:, l, r, :], True, False,
                                   ["qkc", "mCbd"], [pon])
                                yield
                                for hb in range(2):
                                    h_ = 2 * r + hb
                                    mm(po[:, h_ * 65:(h_ + 1) * 65], Wt[:, h_ * 128:(h_ + 1) * 128], Vt[:, h_, :], False,
                                       hb == 1, ["Wt", "Vt"], [pon])
                                    yield
                            pu, pun = pf[1], "pf%d" % (1)
                            for r in range(2):
                                mm(pu[:, r * 130:(r + 1) * 130], k_tm[:, r, :],
                                   Vt[:, 2 * r:2 * r + 2, :].rearrange("p h d -> p (h d)"), True, True, ["k_tm", "Vt"], [pun])
                                yield
                            tt("dve", tmpC[:], mC[:, l, :, :], smm[:, 20:22].unsqueeze(2).to_broadcast([128, 2, 65]), ALU.mult,
                               ["mC", "smm"], ["tmpC"])
                            yield
                            for r in range(2):
                                for hb in range(2):
                                    prt = slice(hb * 64, (hb + 1) * 64)
                                    stt(mC[prt, l, r, :], pu[prt, r * 130 + hb * 65:r * 130 + (hb + 1) * 65],
                                        smm[prt, 20 + r:21 + r], tmpC[prt, r, :], ALU.mult, ALU.add,
                                        [pun, "smm", "tmpC"], ["mC"])
                                    yield
                                    cp("pool", mCbd[prt, l, r, hb * 65:(hb + 1) * 65], mC[prt, l, r, :], ["mC"], ["mCbd"])
                                    yield
                            tt("dve", hr[:], po[:, 0:260].rearrange("p (h d) -> p h d", d=65),
                               smm[:, 12:16].unsqueeze(2).to_broadcast([128, 4, 65]), ALU.mult, [pon, "smm"], ["hr"])
                            yield
                            act(smm[:, 24:28], hr[:, :, 64], AF.Abs, ["hr"], ["smm"])
                            yield
                            ts("dve", smm[:, 24:28], smm[:, 24:28], 1.0, ALU.max, ["smm"], ["smm"])
                            yield
                            recip(smm[:, 24:28], smm[:, 24:28], ["smm"], ["smm"])
                            yield
                            t4 = t256[:].rearrange("p (h d) -> p h d", d=64)
                            u4 = u256[:].rearrange("p (h d) -> p h d", d=64)
                            tt("dve", t4, hr[:, :, 0:64], smm[:, 24:28].unsqueeze(2).to_broadcast([128, 4, 64]), ALU.mult,
                               ["hr", "smm"], ["t256"])
                            yield
                            tt("dve", u4, t4, t4, ALU.mult, ["t256"], ["u256"])
                            yield
                            reduce_add(smm[:, 28:32], u4, ["u256"], ["smm"])
                            yield
                            act(smm[:, 28:32], smm[:, 28:32], AF.Sqrt, ["smm"], ["smm"], bias=EPS, scale=1.0 / 64)
                            yield
                            recip(smm[:, 28:32], smm[:, 28:32], ["smm"], ["smm"])
                            yield
                            tt("dve", t4, t4, smm[:, 28:32].unsqueeze(2).to_broadcast([128, 4, 64]), ALU.mult,
                               ["t256", "smm"], ["t256"])
                            yield
                            tt("dve", t256[:], t256[:], gm_b[:], ALU.mult, ["t256", "gm_b"], ["t256"])
                            yield
                            tt("dve", mix[:, 0:256], t256[:], og[:], ALU.mult, ["t256", "og"], ["mix"])
                            yield


                        def gen_hgrn():
                            c_prev = co
                            gmid = Gc[:, :, co + 32:co + 129:64]
                            gend = Gc[:, :, co + 64:co + 129:64]
                            gprv = Gc[:, :, co:co + 65:64]
                            tt("dve", hgE[:, 0, :, :], gmid, gprv, ALU.subtract, ["Gc"], ["hgE"])
                            yield
                            tt("dve", hgE[:, 1, :, :], gend, gmid, ALU.subtract, ["Gc"], ["hgE"])
                            yield
                            tt("dve", hgE[:, 2, :, :], gend, gprv, ALU.subtract, ["Gc"], ["hgE"])
                            yield
                            act(hgE[:], hgE[:], AF.Exp, ["hgE"], ["hgE"])
                            yield
                            for r in range(2):
                                ts("dve", smh[:, 2 * r:2 * r + 2], Gc[:, r, co + 32:co + 129:64], -1.0, ALU.mult, ["Gc"], ["smh"])
                                yield
                            for r in range(2):
                                for cc in range(2):
                                    cs = co + cc * 64
                                    midc = 1 + cs + 31
                                    act(E1[:], Gc[:, r, 1 + cs:1 + cs + 64], AF.Exp, ["Gc", "smh"], ["E1"],
                                        bias=smh[:, 2 * r + cc:2 * r + cc + 1])
                                    yield
                                    tt("dve", qh[:, r, cc * 64:(cc + 1) * 64], hq_s[:, r, cs:cs + 64], E1[:], ALU.mult,
                                       ["hq_s", "E1"], ["qh"])
                                    yield
                                    act(E2[:], Gc[:, r, 1 + cs:1 + cs + 64], AF.Exp, ["Gc"], ["E2"],
                                        bias=Gc[:, r, midc:midc + 1], scale=-1.0)
                                    yield
                                    tt("dve", kh[:, r, cc * 64:(cc + 1) * 64], key[:, r, cs:cs + 64], E2[:], ALU.mult,
                                       ["key", "E2"], ["kh"])
                                    yield
                                    for hb in range(2):
                                        prt = slice(hb * 64, (hb + 1) * 64)
                                        cp("pool", qbd_h[prt, r, cc, hb * 64:(hb + 1) * 64], qh[prt, r, cc * 64:(cc + 1) * 64],
                                           ["qh"], ["qbd_h"])
                                        yield
                            bk, bkn = bank_b()
                            for r in range(2):
                                tr(bk[:, r * 128:(r + 1) * 128], kh[:, r, :], ident_bf[:], ["kh", "ident_bf"], [bkn])
                            cp("act", kh_tm[:].rearrange("p r d -> p (r d)"), bk[:, 0:256], [bkn], ["kh_tm"])
                            yield
                            yield
                            ps, psn = pf[2], "pf%d" % (2)
                            for cc in range(2):
                                for r in range(2):
                                    mm(ps[cc * 64:(cc + 1) * 64, r * 128:(r + 1) * 128], kh[:, r, cc * 64:(cc + 1) * 64],
                                       qbd_h[:, r, cc, :], True, True, ["kh", "qbd_h"], [psn])
                                    yield
                            tt("dve", At[:], ps[:, 0:256], mask64[:].rearrange("p a b -> p (a b)"), ALU.mult,
                               [psn, "mask64"], ["At"])
                            yield
                            po, pon = pf[3], "pf%d" % (3)
                            Vhs = (Vh0, Vh1)
                            Vhn = ("Vh0", "Vh1")
                            for cc in range(2):
                                cpr = slice(cc * 64, (cc + 1) * 64)
                                for hb in range(2):
                                    prt = slice(hb * 64, (hb + 1) * 64)
                                    tt("dve", hSbd[prt, :, hb * 64:(hb + 1) * 64], hS[prt, l, :, :],
                                       hgE[prt, 0, :, cc:cc + 1].to_broadcast([64, 2, 64]), ALU.mult, ["hS", "hgE"], ["hSbd"])
                                    yield
                                for r in range(2):
                                    mm(po[cpr, r * 128:(r + 1) * 128], qh[:, r, cc * 64:(cc + 1) * 64], hSbd[:, r, :], True, False,
                                       ["qh", "hSbd"], [pon])
                                    yield
                                    for hb in range(2):
                                        h_ = 2 * r + hb
                                        mm(po[cpr, h_ * 64:(h_ + 1) * 64], At[:, h_ * 64:(h_ + 1) * 64],
                                           Vhs[cc][:, h_ * 64:(h_ + 1) * 64], False, hb == 1, ["At", Vhn[cc]], [pon])
                                        yield
                                pu, pun = pf[2], "pf%d" % (2)
                                for r in range(2):
                                    mm(pu[:, r * 128:(r + 1) * 128], kh_tm[:, r, :], Vhs[cc][:, r * 128:(r + 1) * 128], True, True,
                                       ["kh_tm", Vhn[cc]], [pun])
                                    yield
                                tt("dve", tmpS[:], hS[:, l, :, :], hgE[:, 2, :, cc:cc + 1].to_broadcast([128, 2, 64]), ALU.mult,
                                   ["hS", "hgE"], ["tmpS"])
                                yield
                                for r in range(2):
                                    for hb in range(2):
                                        prt = slice(hb * 64, (hb + 1) * 64)
                                        stt(hS[prt, l, r, :], pu[prt, r * 128 + hb * 64:r * 128 + (hb + 1) * 64],
                                            hgE[prt, 1, r, cc:cc + 1], tmpS[prt, r, :], ALU.mult, ALU.add,
                                            [pun, "hgE", "tmpS"], ["hS"])
                                        yield
                            cp("act", t256h[:], po[:, 0:256], [pon], ["t256h"])
                            yield
                            tt("dve", u256h[:], t256h[:], t256h[:], ALU.mult, ["t256h"], ["u256h"])
                            yield
                            reduce_add(smh[:, 8:12], u256h[:].rearrange("p (h d) -> p h d", d=64), ["u256h"], ["smh"])
                            yield
                            act(smh[:, 8:12], smh[:, 8:12], AF.Sqrt, ["smh"], ["smh"], bias=EPS, scale=1.0 / 64)
                            yield
                            recip(smh[:, 8:12], smh[:, 8:12], ["smh"], ["smh"])
                            yield
                            tt("dve", t256h[:].rearrange("p (h d) -> p h d", d=64), t256h[:].rearrange("p (h d) -> p h d", d=64),
                               smh[:, 8:12].unsqueeze(2).to_broadcast([128, 4, 64]), ALU.mult, ["t256h", "smh"], ["t256h"])
                            yield
                            tt("dve", t256h[:], t256h[:], gh_b[:], ALU.mult, ["t256h", "gh_b"], ["t256h"])
                            yield
                            tt("dve", mix[:, 256:512], t256h[:], hgs[:], ALU.mult, ["t256h", "hgs"], ["mix"])
                            yield


                        def gen_swa():
                            tt("dve", sqt[:], sqk[:], sqk[:], ALU.mult, ["sqk"], ["sqt"])
                            yield
                            reduce_add(sms[:, 16:26], sqt[:], ["sqt"], ["sms"])
                            yield
                            act(sms[:, 16:26], sms[:, 16:26], AF.Sqrt, ["sms"], ["sms"], bias=EPS, scale=1.0 / 64)
                            yield
                            recip(sms[:, 16:26], sms[:, 16:26], ["sms"], ["sms"])
                            yield
                            tt("dve", sqk[:], sqk[:], sms[:, 16:26].unsqueeze(2).to_broadcast([128, 10, 64]), ALU.mult,
                               ["sqk", "sms"], ["sqk"])
                            yield
                            tt("dve", sqk[:], sqk[:], gqk_b[:], ALU.mult, ["sqk", "gqk_b"], ["sqk"])
                            yield
                            cosb = rope_c[:, t, :].unsqueeze(1).to_broadcast([128, 10, 8])
                            sinb = rope_s[:, t, :].unsqueeze(1).to_broadcast([128, 10, 8])
                            tt("dve", rtmp[:, 0], sqk[:, :, 0:8], cosb, ALU.mult, ["sqk", "rope_c"], ["rtmp"])
                            yield
                            tt("dve", rtmp[:, 1], sqk[:, :, 8:16], sinb, ALU.mult, ["sqk", "rope_s"], ["rtmp"])
                            yield
                            tt("dve", rtmp[:, 2], sqk[:, :, 8:16], cosb, ALU.mult, ["sqk", "rope_c"], ["rtmp"])
                            yield
                            tt("dve", rtmp[:, 3], sqk[:, :, 0:8], sinb, ALU.mult, ["sqk", "rope_s"], ["rtmp"])
                            yield
                            cp("act", qkr[:], sqk[:], ["sqk"], ["qkr"])
                            yield
                            tt("dve", qkr[:, :, 0:8], rtmp[:, 0], rtmp[:, 1], ALU.subtract, ["rtmp", "qkr"], ["qkr"])
                            yield
                            tt("dve", qkr[:, :, 8:16], rtmp[:, 2], rtmp[:, 3], ALU.add, ["rtmp", "qkr"], ["qkr"])
                            yield
                            bk, bkn = bank_b()
                            qkr2 = qkr[:].rearrange("p h d -> p (h d)")
                            for j in range(5):
                                tr(bk[:, j * 128:(j + 1) * 128], qkr2[:, j * 128:(j + 1) * 128], ident_bf[:], ["qkr", "ident_bf"], [bkn])
                            cp("act", QT0[0:64, :], bk[0:64, 0:512], [bkn], ["QT0"])
                            cp("act", QT1[64:128, :], bk[64:128, 0:512], [bkn], ["QT1"])
                            cp("dve", KT[:, (t + 1) * 128:(t + 2) * 128], bk[:, 512:640], [bkn], ["KT%d" % (t + 1)])
                            yield
                            yield
                            QTs = ((QT0, "QT0"), (QT1, "QT1"))
                            kvsrc = []
                            if gblk > 0:
                                if t == 0:
                                    kvsrc.append((KTcar[:, l, :], "KTcar", Vscar[:, l, :, :], "Vscar", nm_prev, "nm_prev"))
                                else:
                                    kvsrc.append((KT[:, t * 128:(t + 1) * 128], "KT%d" % t, Vs[:, t, :, :], "Vs%d" % t,
                                                  nm_prev, "nm_prev"))
                            kvsrc.append((KT[:, (t + 1) * 128:(t + 2) * 128], "KT%d" % (t + 1), Vs[:, t + 1, :, :],
                                          "Vs%d" % (t + 1), nm_cur, "nm_cur"))
                            pi = 0
                            for hk in range(2):
                                pts = []
                                for ki_, (kap, kn, vap, vn, nmk, nmn) in enumerate(kvsrc):
                                    ps, psn = pf[4 + ki_], "pf%d" % (4 + ki_)
                                    mm(ps[:, :], kap, QTs[hk][0][:, :], True, False, [kn, QTs[hk][1]], [psn])
                                    yield
                                    mm(ps[:, :], ident_bf[:], nmk[:].rearrange("p a b -> p (a b)"), False, True,
                                       ["ident_bf", nmn], [psn])
                                    yield
                                    ptile_ = Pt[pi % 2]
                                    ptn = "Pt%d" % (pi % 2)
                                    pi += 1
                                    act(ptile_[:], ps[:, :], AF.Exp, [psn], [ptn], scale=0.125)
                                    yield
                                    pts.append((ptile_, ptn, vap, vn))
                                po, pon = pf[4], "pf%d" % (4)
                                for g_ in range(4):
                                    for mi_, (ptile_, ptn, vap, vn) in enumerate(pts):
                                        mm(po[:, g_ * 65:(g_ + 1) * 65], ptile_[:, g_ * 128:(g_ + 1) * 128], vap[:, hk, :],
                                           mi_ == 0, mi_ == len(pts) - 1, [ptn, vn], [pon])
                                        yield
                                cp("act", so[:], po[:, 0:260].rearrange("p (h d) -> p h d", d=65), [pon], ["so"])
                                yield
                                tt("dve", sms[:, 32:36], so[:, :, 64], esink[:, hk * 4:(hk + 1) * 4], ALU.add, ["so", "esink"], ["sms"])
                                yield
                                recip(sms[:, 32:36], sms[:, 32:36], ["sms"], ["sms"])
                                yield
                                tt("dve", mix[:, 512 + hk * 256:512 + (hk + 1) * 256].rearrange("p (h d) -> p h d", d=64),
                                   so[:, :, 0:64], sms[:, 32:36].unsqueeze(2).to_broadcast([128, 4, 64]), ALU.mult,
                                   ["so", "sms"], ["mix"])
                                yield
                            if t == T_ - 1:
                                cp("pool", KTcar[:, l, :], KT[:, T_ * 128:(T_ + 1) * 128], ["KT%d" % T_], ["KTcar"])
                                yield
                                cp("pool", Vscar[:, l, :, :], Vs[:, T_, :, :], ["Vs%d" % T_], ["Vscar"])
                                yield


                        gens_ = [gen_mlstm(), gen_hgrn(), gen_swa()]
                        while gens_:
                            for g__ in list(gens_):
                                try:
                                    next(g__)
                                except StopIteration:
                                    gens_.remove(g__)

                        bk, bkn = bank_b()
                        for c in range(KC):
                            tr(bk[:, c * 128:(c + 1) * 128], mix[:, c * 128:(c + 1) * 128], ident_bf[:], ["mix", "ident_bf"], [bkn])
                        cp("act", xn[:], bk[:, :], [bkn], ["xn"])
                        for nh in range(2):
                            ps, psn = bank_f()
                            for c in range(KC):
                                mm(ps[:, :], mixT[:, c, :], sO[:, c, nh * 512:(nh + 1) * 512], c == 0, c == KC - 1,
                                   ["xn", sOn], [psn])
                            tt("dve", xres[:, t, nh * 512:(nh + 1) * 512], xres[:, t, nh * 512:(nh + 1) * 512], ps[:, :],
                               ALU.add, [xr, psn], [xr])
                release(k0 + ("B",))
                release(k0 + ("A1",))
                release(k0 + ("A2",))
                release(k0 + ("O",))

                norm_tiles(list(range(T_)), 1, l, 0)
                for j in range(4):
                    sU, sUn = wslot(k0 + ("U%d" % j,))
                    sD, sDn = wslot(k0 + ("D%d" % j,))
                    for gi in range(T_ // MG):
                        gc0 = gi * MGN
                        for i in range(KC):
                            ps, psn = bank_f()
                            for c in range(KC):
                                mm(ps[:, 0:MGN], sU[:, c, i * 128:(i + 1) * 128], hT[:, c, gc0:gc0 + MGN], c == 0, c == KC - 1,
                                   [sUn, "hT"], [psn])
                            act(sqv[:, 0:MGN], ps[:, 0:MGN], AF.Square, [psn], ["acc"])
                            stt(aT[:, i, :], ps[:, 0:MGN], 0.0, sqv[:, 0:MGN], ALU.is_gt, ALU.mult, [psn, "acc"], ["pre"])
                        for lt in range(MG):
                            t = gi * MG + lt
                            xr = "xres%d" % t
                            for nh in range(2):
                                ps, psn = bank_f()
                                for i in range(KC):
                                    mm(ps[:, :], aT[:, i, lt * 128:(lt + 1) * 128], sD[:, i, nh * 512:(nh + 1) * 512],
                                       i == 0, i == KC - 1, ["pre", sDn], [psn])
                                tt("dve", xres[:, t, nh * 512:(nh + 1) * 512], xres[:, t, nh * 512:(nh + 1) * 512], ps[:, :],
                                   ALU.add, [xr, psn], [xr])
                    release(k0 + ("U%d" % j,))
                    release(k0 + ("D%d" % j,))

                norm_tiles(list(range(T_)), 2, l, 0)
                dma("sp", gpost_b, post_g[l:l + 1, :].partition_broadcast(128), "gpost_b", [], ["key"])
                sG, sGn = wslot(k0 + ("G",))
                sP, sPn = wslot(k0 + ("P",))
                for t in range(T_):
                    xr = "xres%d" % t
                    c0 = t * 128
                    pti = ptile[t % 2]
                    ptn = ("t256", "u256")[t % 2]
                    dma("sp", pti[:], p_d[l, tok0 + c0:tok0 + c0 + 128, :], ptn, [], [ptn])
                    cp("pool", pbf[:], pti[:], [ptn], ["pbf"])
                    bk, bkn = bank_b()
                    for c in range(2):
                        tr(bk[:, c * 128:(c + 1) * 128], pbf[:, c * 128:(c + 1) * 128], ident_bf[:], ["pbf", "ident_bf"], [bkn])
                    cp("act", pT[:].rearrange("p c t -> p (c t)"), bk[:, 0:256], [bkn], ["pT"])
                    pe_banks = []
                    for nh in range(2):
                        ps, psn = bank_f()
                        for c in range(KC):
                            mm(ps[:, :], hT[:, c, c0:c0 + 128], sG[:, c, nh * 512:(nh + 1) * 512], c == 0, c == KC - 1,
                               ["hT", sGn], [psn])
                        act(gate[:, nh * 512:(nh + 1) * 512], ps[:, :], AF.Sigmoid, [psn], ["hq_s"])
                    for nh in range(2):
                        ps, psn = bank_f()
                        for c in range(2):
                            mm(ps[:, :], pT[:, c, :], sP[:, c, nh * 512:(nh + 1) * 512], c == 0, c == 1, ["pT", sPn], [psn])
                        pe_banks.append((ps, psn))
                        act(xn[:, 0:512], ps[:, :], AF.Square, [psn], ["xn", "sm2"], accum=sm2[:, 40 + nh:41 + nh])
                    tt("dve", sm2[:, 42:43], sm2[:, 40:41], sm2[:, 41:42], ALU.add, ["sm2"], ["sm2"])
                    act(sm2[:, 42:43], sm2[:, 42:43], AF.Sqrt, ["sm2"], ["sm2"], bias=EPS, scale=1.0 / D)
                    recip(sm2[:, 42:43], sm2[:, 42:43], ["sm2"], ["sm2"])
                    for nh in range(2):
                        ps, psn = pe_banks[nh]
                        stt(etmp, ps[:, :], sm2[:, 42:43], gpost_b[:, nh * 512:(nh + 1) * 512], ALU.mult, ALU.mult,
                            [psn, "sm2", "key"], ["ftmp"])
                        tt("dve", etmp, etmp, gate[:, nh * 512:(nh + 1) * 512], ALU.mult, ["ftmp", "hq_s"], ["ftmp"])
                        tt("dve", xres[:, t, nh * 512:(nh + 1) * 512], xres[:, t, nh * 512:(nh + 1) * 512], etmp, ALU.add,
                           [xr, "ftmp"], [xr])
                release(k0 + ("G",))
                release(k0 + ("P",))

            for t in range(T_):
                dma("sp", out_d[tok0 + t * 128: tok0 + (t + 1) * 128, :], xres[:, t, :], "st",
                    ["xres%d" % t], ["out%d" % t])
    T.final_wait("sp", ["out%d" % t for t in range(T_)])
    T.emit(nc, st, cfg.get("EPOCH", 32000))
    st.close()
    return nc


_PARAM_NAMES = ["in_norm_g", "w_in", "b_in", "mlstm_f_bias", "mlstm_conv_w", "mlstm_conv_b", "mlstm_norm_g",
                "hgrn_lb_logits", "hgrn_norm_g", "swa_q_norm_g", "swa_k_norm_g", "swa_sinks", "w_out", "mlp_norm_g",
                "w_up", "w_down", "ple_norm_g", "w_ple_gate", "w_ple_proj", "ple_post_norm_g"]


def make_in_maps(inputs, n_cores):
    x = np.asarray(inputs["x"])
    p = np.asarray(inputs["p"])
    pos = np.asarray(inputs["positions"])
    B, S, _ = x.shape
    depth = p.shape[0]
    nseq = B // n_cores
    params = {k: np.ascontiguousarray(np.asarray(inputs[k], dtype=np.float32)) for k in _PARAM_NAMES}
    maps = []
    for c in range(n_cores):
        sl = slice(c * nseq, (c + 1) * nseq)
        m = dict(params)
        m["x"] = np.ascontiguousarray(x[sl].reshape(nseq * S, D))
        m["p"] = np.ascontiguousarray(p[:, sl].reshape(depth, nseq * S, PLE))
        m["positions"] = np.ascontiguousarray(pos[sl].reshape(nseq * S).astype(np.int32))
        maps.append(m)
    return maps, nseq


N_LAUNCH = 1


def kernel(**inputs):
    x = np.asarray(inputs["x"])
    B, S, _ = x.shape
    depth = np.asarray(inputs["p"]).shape[0]
    nseq_total = B // N_CORES
    per = nseq_total // N_LAUNCH
    out = np.zeros((B, S, D), dtype=np.float32)
    cfg = dict(S=S, SEG=min(1024, S), GT=4, DEPTH=depth, NSEQ=per)
    nc = build_program(cfg)
    xs = x.reshape(N_CORES, nseq_total, S, D)
    ps = np.asarray(inputs["p"]).reshape(depth, N_CORES, nseq_total, S, PLE)
    pos = np.asarray(inputs["positions"]).reshape(N_CORES, nseq_total, S)
    for li in range(N_LAUNCH):
        sl = slice(li * per, (li + 1) * per)
        sub = dict(inputs)
        sub["x"] = xs[:, sl].reshape(N_CORES * per, S, D)
        sub["p"] = ps[:, :, sl].reshape(depth, N_CORES * per, S, PLE)
        sub["positions"] = pos[:, sl].reshape(N_CORES * per, S)
        maps, nseq = make_in_maps(sub, N_CORES)
        res = run_bass_kernel_spmd(nc, maps, core_ids=list(range(N_CORES)))
        o = out.reshape(N_CORES, nseq_total, S, D)
        for c, r in enumerate(res.results):
            o[c, sl] = np.asarray(r["out"]).reshape(per, S, D)
    return out
```
